# Optimizing a Trainium2 kernel written in Bass

```python
import jax, jax.numpy as jnp
from jax import lax
import numpy as np

D_MODEL = 2048
BATCH = 4
SEQ = 8192
DEPTH = 2

GRID_W = 64
CTX_LEN = 256
D_MIX = D_MODEL
A_GROUPS = 4
A_GROUP_DIM = 128
A_WIDTH = A_GROUPS * A_GROUP_DIM
A_CHUNK = 128
B_HEADS = 6
B_HEAD_DIM = 128
B_WIDTH = B_HEADS * B_HEAD_DIM
DN_CHUNK = 64
DN_CONV = 5
C_HEADS = 6
C_NOPE = 128
C_ROPE = 64
C_V = 128
C_WIDTH = C_HEADS * C_V
Q_LORA = 512
KV_LORA = 256
Q_BLOCK = 128
ROPE_BASE = 10000.0
ROPE_AXIS_FREQS = C_ROPE // 4
IN_A = 2 * A_WIDTH
IN_B = 4 * B_WIDTH + 4 * B_HEADS
IN_C = Q_LORA + KV_LORA + C_ROPE
IN_COLS = IN_A + IN_B + IN_C
D_FF = 5632
N_EXPERTS = 8
TOP_K = 2
E_FF = 7168
N_DENSE = (DEPTH + 1) // 2
N_MOE = DEPTH // 2
NORM_EPS = 1e-6

kernel_name = 'hybrid_gmlp_deltanet_mla_moe_dit'


def rmsnorm(x, g):
    xf = x.astype(jnp.float32)
    y = xf * lax.rsqrt(jnp.mean(xf * xf, axis=-1, keepdims=True) + NORM_EPS)
    return (y * g.astype(jnp.float32)).astype(x.dtype)


def layernorm(x, g):
    xf = x.astype(jnp.float32)
    xc = xf - jnp.mean(xf, axis=-1, keepdims=True)
    y = xc * lax.rsqrt(jnp.mean(xc * xc, axis=-1, keepdims=True) + NORM_EPS)
    return (y * g.astype(jnp.float32)).astype(x.dtype)


def l2norm(x):
    xf = x.astype(jnp.float32)
    return xf * lax.rsqrt(jnp.sum(xf * xf, axis=-1, keepdims=True) + NORM_EPS)


def modulate(h, shift, scale):
    return h * (1 + scale) + shift


def axial_rope(n):
    rows = n // GRID_W
    row = jnp.repeat(jnp.arange(rows, dtype=jnp.float32), GRID_W)
    col = jnp.tile(jnp.arange(GRID_W, dtype=jnp.float32), rows)
    inv = jnp.power(ROPE_BASE, -jnp.arange(ROPE_AXIS_FREQS, dtype=jnp.float32) / ROPE_AXIS_FREQS)
    ar = row[:, None] * inv
    ac = col[:, None] * inv
    ang = jnp.concatenate([ar, ar, ac, ac], axis=-1)
    return jnp.cos(ang), jnp.sin(ang)


def apply_axial_rope(x, cos, sin):
    r1, r2, c1, c2 = jnp.split(x, 4, axis=-1)
    rot = jnp.concatenate([-r2, r1, -c2, c1], axis=-1)
    return (x.astype(jnp.float32) * cos + rot.astype(jnp.float32) * sin).astype(x.dtype)


def chunk_sgu(za, norm_g, w_s, b_s):
    b, n, _ = za.shape
    z = jax.nn.gelu(za)
    u = z[..., :A_WIDTH]
    v = layernorm(z[..., A_WIDTH:].reshape(b, n, A_GROUPS, A_GROUP_DIM), norm_g.reshape(A_GROUPS, A_GROUP_DIM))
    v = v.reshape(b, n // A_CHUNK, A_CHUNK, A_GROUPS, A_GROUP_DIM)
    mixed = jnp.einsum('gij,bmjgc->bmigc', w_s, v) + b_s.T[None, None, :, :, None]
    return u * mixed.reshape(b, n, A_WIDTH)


def short_conv_silu(x, w):
    n = x.shape[1]
    half = DN_CONV // 2
    xp = jnp.pad(x, ((0, 0), (half, half), (0, 0)))
    y = xp[:, 0:n] * w[0]
    for i in range(1, DN_CONV):
        y = y + xp[:, i:i + n] * w[i]
    return jax.nn.silu(y)


def deltanet_prep(zb, conv_w, a_log, dt_bias):
    b, n, _ = zb.shape
    qkv = short_conv_silu(zb[..., :3 * B_WIDTH], conv_w).reshape(b, n, 3, B_HEADS, B_HEAD_DIM)
    q = l2norm(qkv[:, :, 0])
    k = l2norm(qkv[:, :, 1])
    v = qkv[:, :, 2]
    gate = zb[..., 3 * B_WIDTH:4 * B_WIDTH]
    a = zb[..., 4 * B_WIDTH:4 * B_WIDTH + 2 * B_HEADS].reshape(b, n, 2, B_HEADS).astype(jnp.float32)
    bt = zb[..., 4 * B_WIDTH + 2 * B_HEADS:].reshape(b, n, 2, B_HEADS).astype(jnp.float32)
    g = -jnp.exp(a_log.astype(jnp.float32)) * jax.nn.softplus(a + dt_bias.astype(jnp.float32))
    beta = jax.nn.sigmoid(bt)
    return q, k, v, gate, g, beta


def gated_delta_chunked(q, k, v, g, beta, s0):
    f32 = jnp.float32
    b, n, h, dk = q.shape
    dv = v.shape[-1]
    nc = n // DN_CHUNK

    def chunks(t):
        t = t.astype(f32).reshape((b, nc, DN_CHUNK, h) + t.shape[3:])
        return jnp.moveaxis(t, (1, 3), (0, 2))

    q = chunks(q) * (dk ** -0.5)
    k = chunks(k)
    v = chunks(v)
    beta = chunks(beta)
    gc = jnp.cumsum(chunks(g), axis=-1)
    idx = jnp.arange(DN_CHUNK)
    incl = idx[:, None] >= idx[None, :]
    strict = idx[:, None] > idx[None, :]
    diff = gc[..., :, None] - gc[..., None, :]
    decay = jnp.where(incl, jnp.exp(jnp.where(incl, diff, 0.0)), 0.0)
    kb = k * beta[..., None]
    a_mat = jnp.where(strict, jnp.einsum('nbhik,nbhjk->nbhij', kb, k) * decay, 0.0) + jnp.eye(DN_CHUNK, dtype=f32)
    rhs = jnp.concatenate([v * beta[..., None], kb * jnp.exp(gc)[..., None]], axis=-1)
    sol = lax.linalg.triangular_solve(a_mat, rhs, left_side=True, lower=True, unit_diagonal=True)
    u, w = sol[..., :dv], sol[..., dv:]
    qk = jnp.einsum('nbhik,nbhjk->nbhij', q, k) * decay
    q_g = q * jnp.exp(gc)[..., None]
    k_g = k * jnp.exp(gc[..., -1:] - gc)[..., None]
    g_last = jnp.exp(gc[..., -1])

    def step(s, xs):
        qk_i, q_i, k_i, u_i, w_i, gl_i = xs
        v_new = u_i - jnp.einsum('bhck,bhkv->bhcv', w_i, s)
        o = jnp.einsum('bhck,bhkv->bhcv', q_i, s) + jnp.einsum('bhij,bhjv->bhiv', qk_i, v_new)
        s = s * gl_i[..., None, None] + jnp.einsum('bhck,bhcv->bhkv', k_i, v_new)
        return s, o

    s_fin, o = lax.scan(step, s0.astype(f32), (qk, q_g, k_g, u, w, g_last))
    o = jnp.moveaxis(o, (0, 2), (1, 3)).reshape(b, n, h, dv)
    return o, s_fin


def deltanet_bidir(q, k, v, g, beta, s0_f, s0_b):
    o_f, s_f = gated_delta_chunked(q, k, v, g[:, :, 0], beta[:, :, 0], s0_f)
    fl = lambda t: jnp.flip(t, axis=1)
    o_b, s_b = gated_delta_chunked(fl(q), fl(k), fl(v), fl(g[:, :, 1]), fl(beta[:, :, 1]), s0_b)
    return o_f + fl(o_b), s_f, s_b


def deltanet_out(o, gate, norm_g):
    b, n = o.shape[:2]
    o = rmsnorm(o.astype(gate.dtype), norm_g) * jax.nn.silu(gate.reshape(b, n, B_HEADS, B_HEAD_DIM))
    return o.reshape(b, n, B_WIDTH)


def mla_q(zc, q_norm_g, w_uq, rope):
    b, n, _ = zc.shape
    cq = rmsnorm(zc[..., :Q_LORA], q_norm_g)
    q = (cq @ w_uq).reshape(b, n, C_HEADS, C_NOPE + C_ROPE)
    if rope is not None:
        q_pe = apply_axial_rope(q[..., C_NOPE:], rope[0][:, None, :], rope[1][:, None, :])
        q = jnp.concatenate([q[..., :C_NOPE], q_pe], axis=-1)
    return q


def mla_kv(zc, kv_norm_g, w_ukv, rope):
    b, n, _ = zc.shape
    ckv = rmsnorm(zc[..., Q_LORA:Q_LORA + KV_LORA], kv_norm_g)
    k_pe = zc[..., Q_LORA + KV_LORA:]
    if rope is not None:
        k_pe = apply_axial_rope(k_pe, rope[0], rope[1])
    kv = (ckv @ w_ukv).reshape(b, n, C_HEADS, C_NOPE + C_V)
    k = jnp.concatenate([kv[..., :C_NOPE], jnp.broadcast_to(k_pe[:, :, None, :], (b, n, C_HEADS, C_ROPE))], axis=-1)
    return k, kv[..., C_NOPE:]


def attend(q, k, v):
    s = jnp.einsum('bqhd,bkhd->bhqk', q, k).astype(jnp.float32) * ((C_NOPE + C_ROPE) ** -0.5)
    p = jax.nn.softmax(s, axis=-1).astype(v.dtype)
    return jnp.einsum('bhqk,bkhd->bqhd', p, v)


def attend_blocked(q, k, v):
    b, n, h, d = q.shape
    qb = jnp.moveaxis(q.reshape(b, n // Q_BLOCK, Q_BLOCK, h, d), 1, 0)
    ob = lax.map(lambda qi: attend(qi, k, v), qb)
    return jnp.moveaxis(ob, 0, 1).reshape(b, n, h, -1)


def mixer_sublayer(h, hc, w_in, sgu_norm_g, sgu_w, sgu_b, conv_w, a_log, dt_bias, dn_norm_g,
                   q_norm_g, w_uq, kv_norm_g, w_ukv, w_out, rope, need_ctx):
    b = h.shape[0]
    z = h @ w_in
    za, zb, zl = z[..., :IN_A], z[..., IN_A:IN_A + IN_B], z[..., IN_A + IN_B:]
    if need_ctx:
        zctx = hc @ w_in
        zca = zctx[..., :IN_A]
        zc_bc = zctx[..., IN_A:]
    else:
        zc_bc = hc @ w_in[:, IN_A:]
    zcb, zcc = zc_bc[..., :IN_B], zc_bc[..., IN_B:]
    out_a = chunk_sgu(za, sgu_norm_g, sgu_w, sgu_b)
    s_zero = jnp.zeros((b, B_HEADS, B_HEAD_DIM, B_HEAD_DIM), jnp.float32)
    qc, kc, vc, gatec, gcx, betac = deltanet_prep(zcb, conv_w, a_log, dt_bias)
    oc_b, s_f, s_b = deltanet_bidir(qc, kc, vc, gcx, betac, s_zero, s_zero)
    q, k, v, gate, g, beta = deltanet_prep(zb, conv_w, a_log, dt_bias)
    o_b, _, _ = deltanet_bidir(q, k, v, g, beta, s_f, s_b)
    out_b = deltanet_out(o_b, gate, dn_norm_g)
    k_ctx, v_ctx = mla_kv(zcc, kv_norm_g, w_ukv, None)
    k_lat, v_lat = mla_kv(zl, kv_norm_g, w_ukv, rope)
    q_lat = mla_q(zl, q_norm_g, w_uq, rope)
    out_c = attend_blocked(q_lat, jnp.concatenate([k_lat, k_ctx], axis=1), jnp.concatenate([v_lat, v_ctx], axis=1))
    out_c = out_c.reshape(b, -1, C_WIDTH)
    y = jnp.concatenate([out_a, out_b, out_c], axis=-1) @ w_out
    if not need_ctx:
        return y, None
    oa_c = chunk_sgu(zca, sgu_norm_g, sgu_w, sgu_b)
    ob_c = deltanet_out(oc_b, gatec, dn_norm_g)
    oc_c = attend(mla_q(zcc, q_norm_g, w_uq, None), k_ctx, v_ctx).reshape(b, -1, C_WIDTH)
    yc = jnp.concatenate([oa_c, ob_c, oc_c], axis=-1) @ w_out
    return y, yc


def swiglu(h, wg, wu, wd):
    return (jax.nn.silu(h @ wg) * (h @ wu)) @ wd


def moe_swiglu(h, router, wg, wu, wd):
    logits = (h @ router).astype(jnp.float32)
    probs = jax.nn.softmax(logits, axis=-1)
    top_p, top_i = lax.top_k(probs, TOP_K)
    top_p = top_p / jnp.sum(top_p, axis=-1, keepdims=True)
    flat_e = top_i.reshape(-1)
    order = jnp.argsort(flat_e)
    tok = order // TOP_K
    xs = h[tok]
    sizes = jnp.bincount(flat_e, length=N_EXPERTS).astype(jnp.int32)
    hid = jax.nn.silu(lax.ragged_dot(xs, wg, sizes)) * lax.ragged_dot(xs, wu, sizes)
    ys = lax.ragged_dot(hid, wd, sizes)
    wts = top_p.reshape(-1)[order].astype(ys.dtype)
    return jnp.zeros_like(h).at[tok].add(ys * wts[:, None])


def setup_inputs(seed: int = 0) -> dict:
    key = jax.random.key(seed)
    ks = list(jax.random.split(key, 40))
    f32 = jnp.float32
    def nrm(i, shape, scale):
        return jax.random.normal(ks[i], shape, f32) * scale
    def gain(i, shape):
        return 1.0 + 0.1 * jax.random.normal(ks[i], shape, f32)
    dt = jnp.exp(jax.random.uniform(ks[14], (DEPTH, 2, B_HEADS), f32, np.log(1e-3), np.log(1e-1)))
    return {
        'x': nrm(0, (BATCH, SEQ, D_MODEL), 1.0),
        'c': nrm(1, (BATCH, D_MODEL), 1.0),
        'ctx': nrm(2, (BATCH, CTX_LEN, D_MODEL), 1.0),
        'c_ctx': nrm(3, (D_MODEL,), 1.0),
        'ada_w': nrm(4, (DEPTH, D_MODEL, 6 * D_MODEL), 0.5 * D_MODEL ** -0.5),
        'ada_b': nrm(5, (DEPTH, 6 * D_MODEL), 0.02),
        'norm1_g': gain(6, (DEPTH, D_MODEL)),
        'norm2_g': gain(7, (DEPTH, D_MODEL)),
        'w_in': nrm(8, (DEPTH, D_MODEL, IN_COLS), D_MODEL ** -0.5),
        'sgu_norm_g': gain(9, (DEPTH, A_WIDTH)),
        'sgu_w': nrm(10, (DEPTH, A_GROUPS, A_CHUNK, A_CHUNK), A_CHUNK ** -0.5),
        'sgu_b': nrm(11, (DEPTH, A_GROUPS, A_CHUNK), 0.02),
        'dn_conv_w': nrm(12, (DEPTH, DN_CONV, 3 * B_WIDTH), DN_CONV ** -0.5),
        'dn_a_log': jnp.log(jax.random.uniform(ks[13], (DEPTH, 2, B_HEADS), f32, 1.0, 16.0)),
        'dn_dt_bias': dt + jnp.log(-jnp.expm1(-dt)),
        'dn_norm_g': gain(15, (DEPTH, B_HEAD_DIM)),
        'mla_q_norm_g': gain(16, (DEPTH, Q_LORA)),
        'mla_w_uq': nrm(17, (DEPTH, Q_LORA, C_HEADS * (C_NOPE + C_ROPE)), Q_LORA ** -0.5),
        'mla_kv_norm_g': gain(18, (DEPTH, KV_LORA)),
        'mla_w_ukv': nrm(19, (DEPTH, KV_LORA, C_HEADS * (C_NOPE + C_V)), KV_LORA ** -0.5),
        'w_out': nrm(20, (DEPTH, D_MIX, D_MODEL), D_MIX ** -0.5),
        'ffn_w_gate': nrm(21, (N_DENSE, D_MODEL, D_FF), D_MODEL ** -0.5),
        'ffn_w_up': nrm(22, (N_DENSE, D_MODEL, D_FF), D_MODEL ** -0.5),
        'ffn_w_down': nrm(23, (N_DENSE, D_FF, D_MODEL), D_FF ** -0.5),
        'moe_router': nrm(24, (N_MOE, D_MODEL, N_EXPERTS), D_MODEL ** -0.5),
        'moe_w_gate': nrm(25, (N_MOE, N_EXPERTS, D_MODEL, E_FF), D_MODEL ** -0.5),
        'moe_w_up': nrm(26, (N_MOE, N_EXPERTS, D_MODEL, E_FF), D_MODEL ** -0.5),
        'moe_w_down': nrm(27, (N_MOE, N_EXPERTS, E_FF, D_MODEL), E_FF ** -0.5),
        'final_norm_g': gain(28, (D_MODEL,)),
    }


def reference(x, c, ctx, c_ctx, ada_w, ada_b, norm1_g, norm2_g, w_in, sgu_norm_g, sgu_w, sgu_b,
              dn_conv_w, dn_a_log, dn_dt_bias, dn_norm_g, mla_q_norm_g, mla_w_uq, mla_kv_norm_g, mla_w_ukv,
              w_out, ffn_w_gate, ffn_w_up, ffn_w_down, moe_router, moe_w_gate, moe_w_up, moe_w_down,
              final_norm_g):
    b, n, d = x.shape
    lc = ctx.shape[1]
    rope = axial_rope(n)
    xc = ctx
    for i in range(DEPTH):
        last = i == DEPTH - 1
        mod = jax.nn.silu(c) @ ada_w[i] + ada_b[i]
        mod_c = jax.nn.silu(c_ctx) @ ada_w[i] + ada_b[i]
        sh1, sc1, g1, sh2, sc2, g2 = jnp.split(mod[:, None, :], 6, axis=-1)
        sh1c, sc1c, g1c, sh2c, sc2c, g2c = jnp.split(mod_c, 6, axis=-1)
        h = modulate(rmsnorm(x, norm1_g[i]), sh1, sc1)
        hc = modulate(rmsnorm(xc, norm1_g[i]), sh1c, sc1c)
        y, yc = mixer_sublayer(h, hc, w_in[i], sgu_norm_g[i], sgu_w[i], sgu_b[i], dn_conv_w[i], dn_a_log[i],
                               dn_dt_bias[i], dn_norm_g[i], mla_q_norm_g[i], mla_w_uq[i], mla_kv_norm_g[i],
                               mla_w_ukv[i], w_out[i], rope, not last)
        x = x + g1 * y
        h = modulate(rmsnorm(x, norm2_g[i]), sh2, sc2).reshape(b * n, d)
        if not last:
            xc = xc + g1c * yc
            hc = modulate(rmsnorm(xc, norm2_g[i]), sh2c, sc2c).reshape(b * lc, d)
            h = jnp.concatenate([h, hc], axis=0)
        if i % 2 == 0:
            f = swiglu(h, ffn_w_gate[i // 2], ffn_w_up[i // 2], ffn_w_down[i // 2])
        else:
            f = moe_swiglu(h, moe_router[i // 2], moe_w_gate[i // 2], moe_w_up[i // 2], moe_w_down[i // 2])
        x = x + g2 * f[:b * n].reshape(b, n, d)
        if not last:
            xc = xc + g2c * f[b * n:].reshape(b, lc, d)
    return rmsnorm(x, final_norm_g)
```

```python
import numpy as np
from contextlib import ExitStack
import concourse.bass as bass
import concourse.mybir as mybir
from concourse.bass_utils import run_bass_kernel_spmd

F32 = mybir.dt.float32
BF16 = mybir.dt.bfloat16
ALU = mybir.AluOpType
AF = mybir.ActivationFunctionType
AX = mybir.AxisListType

EPOCH = 30000
NDS = 6


class TAP:
    __slots__ = ("tile", "ap")

    def __init__(self, tile, ap):
        self.tile = tile
        self.ap = ap

    def __getitem__(self, idx):
        return TAP(self.tile, self.ap[idx])

    def rearrange(self, *a, **k):
        return TAP(self.tile, self.ap.rearrange(*a, **k))


class Tile:
    __slots__ = ("t", "w", "r", "name", "psum")

    def __init__(self, t, name, psum=False):
        self.t = t
        self.w = None
        self.r = []
        self.name = name
        self.psum = psum

    def __getitem__(self, idx):
        return TAP(self, self.t[idx])


class Rec:
    __slots__ = ("eng", "fn", "dma", "deps", "signal", "sigval", "idx", "dn", "raw")

    def __init__(self, eng, fn, dma):
        self.eng = eng
        self.fn = fn
        self.dma = dma
        self.deps = ()
        self.raw = ()
        self.signal = False
        self.sigval = 0
        self.idx = 0
        self.dn = 0


WRITE_KEYS = ("out", "accum_out")
PTR_KEYS = ("scalar", "scalar1", "scalar2", "scale", "bias")


class Prog:
    ENG = ["pe", "act", "dve", "pool", "sp"]

    def __init__(self, nc, same_eng_sync=True):
        self.nc = nc
        self.es = ExitStack()
        self.ins = {e: [] for e in self.ENG}
        self.ndma = {e: 0 for e in self.ENG}
        self.dmarecs = {e: [] for e in self.ENG}
        self.same_eng_sync = same_eng_sync
        self.uid = 0
        self.pending = {e: set() for e in self.ENG}

    def sb(self, shape, dtype=F32, name=None, es=None):
        self.uid += 1
        name = name or f"sb{self.uid}"
        t = (es or self.es).enter_context(self.nc.sbuf_tensor(name, list(shape), dtype))
        return Tile(t, name)

    def ps(self, shape, dtype=F32, name=None, es=None):
        self.uid += 1
        name = name or f"ps{self.uid}"
        t = (es or self.es).enter_context(self.nc.psum_tensor(name, list(shape), dtype))
        return Tile(t, name, psum=True)

    def dram(self, name, shape, dtype, kind="Internal"):
        t = self.nc.dram_tensor(name, list(shape), dtype, kind=kind)
        return Tile(t.ap(), name)

    def view(self, ap, name="v"):
        return Tile(ap, name)

    def op(self, eng, fn, reads=(), writes=(), dma=False, ptr_reads=()):
        rec = Rec(eng, fn, dma)
        lst = self.ins[eng]
        rec.idx = len(lst)
        deps = set()
        for r in reads:
            if r.w is not None:
                deps.add(r.w)
        rec.raw = set(deps)
        for r in reads:
            if r.psum and r.r:
                best = {}
                for rr in r.r:
                    if rr.eng != eng:
                        b = best.get(rr.eng)
                        if b is None or rr.idx > b.idx:
                            best[rr.eng] = rr
                deps.update(best.values())
        for w in writes:
            if w.w is not None:
                deps.add(w.w)
            if w.r:
                best = {}
                for rr in w.r:
                    if rr.dma:
                        deps.add(rr)
                    else:
                        b = best.get(rr.eng)
                        if b is None or rr.idx > b.idx:
                            best[rr.eng] = rr
                deps.update(best.values())
        if self.pending[eng]:
            deps.update(self.pending[eng])
            self.pending[eng] = set()
        deps.discard(rec)
        rec.deps = deps
        for r in reads:
            r.r.append(rec)
        for w in writes:
            w.w = rec
            w.r = []
        if dma:
            rec.dn = self.ndma[eng]
            self.ndma[eng] += 1
            self.dmarecs[eng].append(rec)
        lst.append(rec)
        return rec

    def I(self, eng, meth, *args, **kw):
        reads, writes, ptrs = [], [], []
        kw2 = {}
        for k, v in kw.items():
            if isinstance(v, TAP):
                tl = v.tile if isinstance(v.tile, tuple) else (v.tile,)
                if k in WRITE_KEYS:
                    writes.extend(tl)
                    if k == "accum_out":
                        reads.extend(tl)
                else:
                    reads.extend(tl)
                kw2[k] = v.ap
            else:
                kw2[k] = v
        return self.op(eng, lambda e: getattr(e, meth)(**kw2), reads, writes, ptr_reads=ptrs)

    def dma(self, out, in_, q="sp", **kw):
        reads, writes = [], []
        if isinstance(out, TAP):
            writes.extend(out.tile if isinstance(out.tile, tuple) else (out.tile,))
            out = out.ap
        if isinstance(in_, TAP):
            reads.extend(in_.tile if isinstance(in_.tile, tuple) else (in_.tile,))
            in_ = in_.ap
        return self.op(q, lambda e: e.dma_start(out=out, in_=in_, **kw), reads, writes, dma=True)

    def mm(self, out, lhsT, rhs, start=True, stop=True, extra_reads=()):
        reads = list(extra_reads)
        for t in (lhsT.tile, rhs.tile):
            reads.extend(t if isinstance(t, tuple) else (t,))
        o, l, r = out.ap, lhsT.ap, rhs.ap
        return self.op("pe", lambda e: e.matmul(o, l, r, start=start, stop=stop), reads, [out.tile])

    def tr(self, out, in_, ident):
        o, i, d = out.ap, in_.ap, ident.ap
        rd = list(in_.tile if isinstance(in_.tile, tuple) else (in_.tile,)) + [ident.tile]
        return self.op("pe", lambda e: e.transpose(out=o, in_=i, identity=d), rd, [out.tile])

    def barrier(self):
        last = set()
        for e in self.ENG:
            if self.ins[e]:
                last.add(self.ins[e][-1])
            for rec in self.dmarecs[e][-NDS:]:
                last.add(rec)
        for e in self.ENG:
            self.pending[e] = set(last)

    def _needs_wait(self, rec, d):
        if d.dma:
            return True
        if d.eng == rec.eng:
            if rec.dma:
                return True
            if rec.eng == "pe":
                return False
            return self.same_eng_sync
        return True

    def finish(self):
        nc = self.nc
        for e in self.ENG:
            for rec in self.ins[e]:
                for d in rec.deps:
                    if (not d.dma) and self._needs_wait(rec, d):
                        d.signal = True
        nsig = {}
        for e in self.ENG:
            c = 0
            for rec in self.ins[e]:
                if rec.signal:
                    c += 1
                    rec.sigval = c
            nsig[e] = c
        self.nsig = nsig
        sems = {}
        for e in self.ENG:
            n_ep = (nsig[e] + EPOCH - 1) // EPOCH
            sems[e] = [self.es.enter_context(nc.semaphore(f"s_{e}_{i}")) for i in range(max(n_ep, 1))]
        dsems = {}
        for e in self.ENG:
            if self.ndma[e]:
                dsems[e] = [self.es.enter_context(nc.semaphore(f"d_{e}_{i}")) for i in range(NDS)]

        def emit(e, eng):
            waited = {x: 0 for x in self.ENG}
            dwaited = {}
            for rec in self.ins[e]:
                need = {}
                for d in rec.deps:
                    if not self._needs_wait(rec, d):
                        continue
                    if d.dma:
                        key = (d.eng, d.dn % NDS)
                        val = 16 * (d.dn // NDS + 1)
                        if dwaited.get(key, 0) < val:
                            dwaited[key] = val
                            eng.wait_ge(dsems[d.eng][d.dn % NDS], val)
                    else:
                        if d.sigval > need.get(d.eng, 0):
                            need[d.eng] = d.sigval
                for x, sv in need.items():
                    if waited[x] < sv:
                        waited[x] = sv
                        eng.wait_ge(sems[x][(sv - 1) // EPOCH], (sv - 1) % EPOCH + 1)
                if rec.dma and rec.dn >= NDS:
                    key = (e, rec.dn % NDS)
                    val = 16 * (rec.dn // NDS)
                    if dwaited.get(key, 0) < val:
                        dwaited[key] = val
                        eng.wait_ge(dsems[e][rec.dn % NDS], val)
                ins = rec.fn(eng)
                if rec.dma:
                    ins.then_inc(dsems[e][rec.dn % NDS], 16)
                elif rec.signal:
                    ins.then_inc(sems[e][(rec.sigval - 1) // EPOCH], 1)
            n = self.ndma[e]
            if n:
                for k in range(NDS):
                    cnt = (n - k + NDS - 1) // NDS if n > k else 0
                    if cnt:
                        eng.wait_ge(dsems[e][k], 16 * cnt)

        with nc.Block() as block:
            if self.ins["pe"]:
                @block.tensor
                def _(eng):
                    emit("pe", eng)
            if self.ins["act"]:
                @block.scalar
                def _(eng):
                    emit("act", eng)
            if self.ins["dve"]:
                @block.vector
                def _(eng):
                    emit("dve", eng)
            if self.ins["pool"]:
                @block.gpsimd
                def _(eng):
                    emit("pool", eng)
            if self.ins["sp"]:
                @block.sync
                def _(eng):
                    emit("sp", eng)
        self.es.close()

import os

D = 2048
SEQ = 8192
CTX = 256
TB = 256
NB = TB + 4
NBLK = 1 + SEQ // TB
NCOL = NB + SEQ + 4
NTOK = CTX + SEQ
IN_A = 1024
IN_B = 4 * 768 + 24
A_W = 512
EPS = 1e-6
NFM = 30


def blk_info(n):
    if n == 0:
        return 0, 0, True, True, True
    m = n - 1
    return NB + TB * m, CTX + TB * m, False, m == 0, m == SEQ // TB - 1


def make_consts():
    c = {}
    c["ones"] = np.ones((128, 128), np.float32)
    c["ident"] = np.eye(128, dtype=np.float32)
    k = np.arange(64)
    tri = (k[:, None] <= k[None, :]).astype(np.float32)
    c["tri"] = tri
    low_incl = (k[:, None] >= k[None, :]).astype(np.float32)
    low_strict = (k[:, None] > k[None, :]).astype(np.float32)
    up_incl = (k[:, None] <= k[None, :]).astype(np.float32)
    c["m_low"] = np.tile(low_incl[:, None, :], (1, 6, 1))
    c["m_lows"] = np.tile(low_strict[:, None, :], (1, 6, 1))
    c["m_up"] = np.tile(up_incl[:, None, :], (1, 6, 1))
    c["id6"] = np.tile(np.eye(64, dtype=np.float32)[:, None, :], (1, 6, 1))
    return c


def rope_tables():
    row = np.repeat(np.arange(SEQ // 64, dtype=np.float32), 64)
    col = np.tile(np.arange(64, dtype=np.float32), SEQ // 64)
    inv = np.power(np.float32(10000.0), -np.arange(16, dtype=np.float32) / np.float32(16)).astype(np.float32)
    ar = row[:, None] * inv
    ac = col[:, None] * inv
    ang = np.concatenate([ar, ar, ac, ac], axis=-1)
    cos = np.cos(ang).astype(np.float32)
    sin = np.sin(ang).astype(np.float32)
    sgn = np.concatenate([-np.ones(16), np.ones(16), -np.ones(16), np.ones(16)]).astype(np.float32)
    return cos, sin * sgn


SWAP = np.concatenate([np.arange(16, 32), np.arange(0, 16), np.arange(48, 64), np.arange(32, 48)])


def mix_host(inp, li, r, xfull, ctxfull, xT_ready=None):
    b, dr = r // 2, r % 2
    f32 = np.float32
    if xT_ready is not None:
        xT = xT_ready
    else:
        xs = xfull[b]
        cs = ctxfull[b]
        if dr:
            xs = xs[::-1]
            cs = cs[::-1]
        xT = np.zeros((D, NCOL), f32)
        xT[:, 2:2 + CTX] = cs.T
        xT[:, NB + 2:NB + 2 + SEQ] = xs.T
    m = {"xT": xT}
    m["c2T"] = np.ascontiguousarray(np.stack([inp["c"][b], inp["c_ctx"]], axis=1))
    m["ada_w"] = np.ascontiguousarray(inp["ada_w"][li][:, :2 * D])
    m["ada_bT"] = np.ascontiguousarray(inp["ada_b"][li][:2 * D].reshape(32, 128).T)
    m["n1g"] = np.ascontiguousarray(inp["norm1_g"][li].reshape(16, 128).T)
    w_in = inp["w_in"][li]
    gs = [2 * dr, 2 * dr + 1]
    hs = [3 * dr + i for i in range(3)]
    cols = []
    for g in gs:
        cols.append(np.arange(g * 128, (g + 1) * 128))
    cols.append(IN_A + np.arange(2304))
    for h in hs:
        cols.append(IN_A + 3 * 768 + h * 128 + np.arange(128))
    cols.append(IN_A + IN_B + np.arange(768))
    cols.append(IN_A + IN_B + 768 + np.arange(64))
    cols.append(IN_A + IN_B + 768 + SWAP)
    cols = np.concatenate(cols)
    assert cols.shape[0] == NFM * 128
    m["w_fm"] = np.ascontiguousarray(w_in[:, cols])
    tcols = []
    for g in gs:
        tcols.append(A_W + g * 128 + np.arange(128))
    tcols.append(IN_A + 4 * 768 + dr * 6 + np.arange(6))
    tcols.append(IN_A + 4 * 768 + 12 + dr * 6 + np.arange(6))
    m["w_tm"] = np.ascontiguousarray(w_in[:, np.concatenate(tcols)])
    sg = inp["sgu_norm_g"][li].reshape(4, 128)[gs].reshape(1, 256)
    m["sgu_g"] = np.ascontiguousarray(np.broadcast_to(sg, (128, 256)))
    ws = inp["sgu_w"][li][gs]
    bs = inp["sgu_b"][li][gs]
    if dr:
        ws = ws[:, ::-1, ::-1]
        bs = bs[:, ::-1]
    m["wsT"] = np.ascontiguousarray(np.transpose(ws, (2, 0, 1)))
    m["bs"] = np.ascontiguousarray(bs.reshape(1, 2, 128))
    cw = inp["dn_conv_w"][li]
    if dr:
        cw = cw[::-1]
    m["convw"] = np.ascontiguousarray(cw.reshape(5, 18, 128).transpose(2, 1, 0))
    m["alog"] = np.ascontiguousarray(np.broadcast_to(inp["dn_a_log"][li][dr].reshape(1, 1, 6), (64, 4, 6)))
    m["dtb"] = np.ascontiguousarray(np.broadcast_to(inp["dn_dt_bias"][li][dr].reshape(1, 1, 6), (64, 4, 6)))
    m["qng"] = np.ascontiguousarray(inp["mla_q_norm_g"][li].reshape(4, 128).T)
    m["kvng"] = np.ascontiguousarray(inp["mla_kv_norm_g"][li].reshape(2, 128).T)
    wuq = inp["mla_w_uq"][li]
    qc = []
    for h in hs:
        qc.append(h * 192 + np.arange(128))
        qc.append(h * 192 + 128 + np.arange(64))
        qc.append(h * 192 + 128 + SWAP)
    m["wuq"] = np.ascontiguousarray(wuq[:, np.concatenate(qc)])
    wukv = inp["mla_w_ukv"][li]
    kc = [h * 256 + np.arange(128) for h in hs] + [h * 256 + 128 + np.arange(128) for h in hs]
    m["wukv"] = np.ascontiguousarray(wukv[:, np.concatenate(kc)])
    cos, sins = rope_tables()
    if dr:
        cos = cos[::-1]
        sins = sins[::-1]
    cT = np.ones((64, NTOK), f32)
    sT = np.zeros((64, NTOK), f32)
    cT[:, CTX:] = cos.T
    sT[:, CTX:] = sins.T
    m["cosT"] = cT
    m["sinT"] = sT
    for k, v in make_consts().items():
        m["c_" + k] = v
    return m


QB = 512
NKT = NTOK // 128
ASCALE = 192.0 ** -0.5


def build_mix(stages=("sgu", "dn", "mla", "attn"), nblk=NBLK):
    nc = bass.Bass("TRN2", target_bir_lowering=False)
    p = Prog(nc)

    def din(name, shape):
        return nc.dram_tensor(name, list(shape), F32, kind="ExternalInput").ap()

    def dout(name, shape):
        return nc.dram_tensor(name, list(shape), F32, kind="ExternalOutput").ap()

    xT = din("xT", [D, NCOL])
    c2T = din("c2T", [D, 2])
    ada_w = din("ada_w", [D, 2 * D])
    ada_bT = din("ada_bT", [128, 32])
    n1g = din("n1g", [128, 16])
    w_fm = din("w_fm", [D, NFM * 128])
    w_tm = din("w_tm", [D, 268])
    sgu_g = din("sgu_g", [128, 256])
    wsT_d = din("wsT", [128, 2, 128])
    bs_d = din("bs", [1, 2, 128])
    convw_d = din("convw", [128, 18, 5])
    alog_d = din("alog", [64, 4, 6])
    dtb_d = din("dtb", [64, 4, 6])
    qng_d = din("qng", [128, 4])
    kvng_d = din("kvng", [128, 2])
    wuq_d = din("wuq", [512, 768])
    wukv_d = din("wukv", [256, 768])
    cosT_d = din("cosT", [64, NTOK])
    sinT_d = din("sinT", [64, NTOK])
    c_ones = din("c_ones", [128, 128])
    c_ident = din("c_ident", [128, 128])
    c_tri = din("c_tri", [64, 64])
    c_mlow = din("c_m_low", [64, 6, 64])
    c_mlows = din("c_m_lows", [64, 6, 64])
    c_mup = din("c_m_up", [64, 6, 64])
    c_id6 = din("c_id6", [64, 6, 64])

    outA = dout("outA", [256, NTOK])
    oT = dout("oT", [768, NTOK])
    sgate = dout("sgate", [384, NTOK])
    outC = dout("outC", [384, NTOK])
    dbg = dout("dbg", [128, 64, 260]) if any(st_.startswith("dbg") for st_ in stages) else None

    Ksn = nc.dram_tensor("Ksn", [128, 3, NTOK], BF16, kind="Internal").ap()
    Ksp = nc.dram_tensor("Ksp", [64, NTOK], BF16, kind="Internal").ap()
    Vsc = nc.dram_tensor("Vsc", [128, NKT, 384], BF16, kind="Internal").ap()
    Qsn = nc.dram_tensor("Qsn", [128, 3, NTOK], BF16, kind="Internal").ap()
    Qsp = nc.dram_tensor("Qsp", [64, 3, NTOK], BF16, kind="Internal").ap()

    ones_bf = p.sb([128, 128], BF16)
    p.dma(ones_bf[:], c_ones, q="pool")
    ident = p.sb([128, 128])
    p.dma(ident[:], c_ident)
    epsb = p.sb([128, 1])
    p.op("dve", lambda e: e.memset(epsb[:].ap, EPS), [], [epsb])
    PS = [p.ps([128, 512]) for _ in range(8)]

    es1 = ExitStack()

    def sb(shape, dtype=F32):
        return p.sb(shape, dtype, es=es1)

    ones_f = sb([128, 128])
    p.dma(ones_f[:], c_ones)
    tri = sb([64, 64])
    p.dma(tri[:], c_tri)
    m_lows = sb([64, 3, 64])
    p.dma(m_lows[:], c_mlows[:, 0:3, :])
    m_up = sb([64, 3, 64])
    p.dma(m_up[:], c_mup[:, 0:3, :])
    id3 = sb([64, 3, 64])
    p.dma(id3[:], c_id6[:, 0:3, :])
    cw = sb([128, 18, 5])
    p.dma(cw[:], convw_d)
    alog = sb([64, 4, 6])
    p.dma(alog[:], alog_d)
    dtb = sb([64, 4, 6])
    p.dma(dtb[:], dtb_d)
    nea = sb([64, 4, 6])
    p.I("act", "activation", out=nea[:], in_=alog[:], func=AF.Exp)
    p.I("dve", "tensor_scalar", out=nea[:], in0=nea[:], scalar1=-1.0, scalar2=None, op0=ALU.mult)
    qng = sb([128, 4])
    p.dma(qng[:], qng_d)
    kvng = sb([128, 2])
    p.dma(kvng[:], kvng_d)
    wuq = sb([128, 4, 768], BF16)
    p.dma(wuq[:], wuq_d.rearrange("(c p) n -> p c n", p=128), q="pool")
    wukv = sb([128, 2, 768], BF16)
    p.dma(wukv[:], wukv_d.rearrange("(c p) n -> p c n", p=128), q="pool")

    c2 = sb([128, 16, 2])
    p.dma(c2[:], c2T.rearrange("(c p) n -> p c n", p=128))
    sc2 = sb([128, 16, 2])
    p.I("act", "activation", out=sc2[:], in_=c2[:], func=AF.Silu)
    adab = sb([128, 32])
    p.dma(adab[:], ada_bT)
    n1 = sb([128, 16])
    p.dma(n1[:], n1g)
    modT = sb([128, 32, 2])
    qkv = sb([128, 18, TB])
    awt = [qkv[:, 0:8, :].rearrange("p a (b c) -> p (a b) c", c=128), qkv[:, 8:16, :].rearrange("p a (b c) -> p (a b) c", c=128)]
    BC = PS[0]
    for j in range(32):
        w = awt[j % 2]
        p.dma(w, ada_w[:, j * 128:(j + 1) * 128].rearrange("(c p) n -> p c n", p=128))
        for k in range(16):
            p.mm(BC[:, 2 * (j % 2):2 * (j % 2) + 2], w[:, k, :], sc2[:, k, :], k == 0, k == 15)
        p.I("dve", "tensor_scalar", out=modT[:, j, :], in0=BC[:, 2 * (j % 2):2 * (j % 2) + 2], scalar1=adab[:, j:j + 1],
            scalar2=None, op0=ALU.add)
    gsT = sb([128, 16, 2])
    for s in range(2):
        p.I("dve", "scalar_tensor_tensor", out=gsT[:, :, s], in0=modT[:, 16:32, s], scalar=1.0, in1=n1[:], op0=ALU.add, op1=ALU.mult)

    wtm = sb([128, 16, 268], BF16)
    p.dma(wtm[:], w_tm.rearrange("(c p) n -> p c n", p=128), q="pool")
    sgg = sb([128, 256])
    p.dma(sgg[:], sgu_g)
    wsT = sb([128, 2, 128], BF16)
    p.dma(wsT[:], wsT_d, q="pool")
    bsb = sb([1, 2, 128], BF16)
    p.dma(bsb[:], bs_d, q="pool")

    xt = [sb([128, NB]) for _ in range(4)]
    hT = sb([128, 16, NB], BF16)
    sqb = [sb([128, NB], BF16) for _ in range(2)]
    tmpf = [sb([128, NB]) for _ in range(2)]
    rstd = sb([128, NB])
    wt = [sb([128, 16, 128], BF16) for _ in range(3)]
    Z = [PS[1], PS[2]]
    TMp = PS[3]
    W = PS[4:8]
    wrot = [0]

    def nxt():
        w = W[wrot[0] % 4]
        wrot[0] += 1
        return w
    uT = sb([128, 2, TB])
    zq = sb([128, 18, NB], BF16)
    sg = sb([128, 3, TB])
    ql = sb([128, 4, TB])
    kvl = sb([128, 2, TB])
    kp = sb([64, 2, TB])
    gv = sb([128, 256])
    cen = sb([128, 256])
    junk = sb([128, 128])
    st1 = sb([128, 8])
    vn = sb([128, 256], BF16)
    oa = sb([128, 2, TB])
    sqq = sb([128, 2, TB], BF16)
    rinv = sb([128, 2, TB])
    ab4 = sb([64, 4, 12])
    g4 = sb([64, 4, 6])
    beta4 = sb([64, 4, 6])
    osb = sb([128, 6, TB])
    Sf = [sb([128, 3, 128]) for _ in range(2)]
    Sb = [sb([128, 3, 128], BF16) for _ in range(2)]
    for hg in range(2):
        p.op("pool", lambda e, t=Sf[hg]: e.memset(t[:].ap, 0.0), [], [Sf[hg]])
        p.op("pool", lambda e, t=Sb[hg]: e.memset(t[:].ap, 0.0), [], [Sb[hg]])

    def dset():
        d = {}
        d["ktm"] = sb([64, 3, 128]); d["vtm"] = sb([64, 3, 128])
        d["Pm"] = sb([64, 3, 64]); d["gcol"] = sb([64, 3]); d["EGB"] = sb([128, 3, 64])
        d["D1s"] = sb([64, 3, 64]); d["D2"] = sb([64, 3, 64])
        d["egc"] = sb([64, 3]); d["be"] = sb([64, 3]); d["nbeta"] = sb([64, 3]); d["dl"] = sb([64, 3]); d["ekl"] = sb([64, 3])
        d["Msb"] = sb([64, 3, 64]); d["qkT"] = sb([64, 3, 64], BF16)
        d["PP"] = [sb([64, 2, 3, 64]) for _ in range(2)]
        d["TT"] = [sb([64, 3, 64]) for _ in range(2)]
        d["vb"] = sb([64, 3, 128]); d["kbg"] = sb([64, 3, 128]); d["kg"] = sb([64, 3, 128], BF16)
        d["u"] = sb([64, 3, 128]); d["wT"] = sb([128, 3, 64], BF16); d["qgT"] = sb([128, 3, 64], BF16)
        d["vnew"] = sb([64, 3, 128], BF16)
        return d
    dnl = 0
    for st_ in stages:
        if st_ == "dn":
            dnl = 9
        elif st_.startswith("dn:"):
            dnl = int(st_[3:])
    if dnl:
        stages = tuple(stages) + ("dn",)
    DS = [dset() for hg in range(2)] if "dn" in stages else None
    if "mla" in stages:
        sq4 = sb([128, 4, TB], BF16)
        rq = sb([128, TB])
        cqT = sb([128, 4, TB], BF16)
        ckvT = sb([128, 2, TB], BF16)
        cosb = sb([64, TB]); sinb = sb([64, TB])
        qn_sb = sb([128, 3, TB], BF16); qp_sb = sb([64, 3, TB], BF16)
        kn_sb = sb([128, 3, TB], BF16); kp_sb = sb([64, TB], BF16)
        v_sb = sb([128, 2, 384], BF16)
        t1 = sb([64, TB]); t2 = sb([64, TB])
    p.barrier()
    wcnt = 0

    for n in range(nblk):
        c0, t0, is_ctx, first, last = blk_info(n)
        s = 1 if is_ctx else 0
        h = hT
        for c in range(16):
            x = xt[c % 4]
            p.dma(x[:], xT[c * 128:(c + 1) * 128, c0:c0 + NB])
            sq = sqb[c % 2]
            p.I("act", "activation", out=sq[:], in_=x[:], func=AF.Square)
            p.mm(BC[:, 0:NB], ones_bf[:], sq[:], c == 0, c == 15)
        p.I("act", "activation", out=rstd[:], in_=BC[:, 0:NB], func=AF.Sqrt, scale=1.0 / D, bias=epsb[:])
        p.I("dve", "reciprocal", out=rstd[:], in_=rstd[:])
        for c in range(16):
            x = xt[c % 4]
            p.dma(x[:], xT[c * 128:(c + 1) * 128, c0:c0 + NB])
            tf = tmpf[c % 2]
            p.I("dve", "tensor_tensor", out=tf[:], in0=x[:], in1=rstd[:], op=ALU.mult)
            p.I("act", "activation", out=h[:, c, :], in_=tf[:], func=AF.Identity, scale=gsT[:, c, s:s + 1], bias=modT[:, c, s:s + 1])
        if first:
            p.op("pool", lambda e, h=h: e.memset(h[:, :, 0:2].ap, 0.0), [], [h])
        if last:
            p.op("pool", lambda e, h=h: e.memset(h[:, :, NB - 2:NB].ap, 0.0), [], [h])
        for j in range(NFM):
            w = wt[wcnt % 3]
            wcnt += 1
            p.dma(w[:], w_fm[:, j * 128:(j + 1) * 128].rearrange("(c p) n -> p c n", p=128), q="pool")
            zp = Z[j % 2]
            if j < NFM - 1:
                for k in range(16):
                    p.mm(zp[:, 0:NB], w[:, k, :], h[:, k, :], k == 0, k == 15)
            else:
                for half in range(2):
                    for k in range(16):
                        p.mm(zp[0:64, half * TB:(half + 1) * TB], w[:, k, half * 64:(half + 1) * 64], h[:, k, 2:2 + TB], k == 0, k == 15)
            if j < 2:
                p.I("act", "activation", out=uT[:, j, :], in_=zp[:, 2:2 + TB], func=AF.Gelu_apprx_tanh)
            elif j < 20:
                p.I("act", "activation", out=zq[:, j - 2, :], in_=zp[:, 0:NB], func=AF.Identity)
            elif j < 23:
                p.I("act", "activation", out=sg[:, j - 20, :], in_=zp[:, 2:2 + TB], func=AF.Silu)
            elif j < 27:
                p.I("dve", "tensor_copy", out=ql[:, j - 23, :], in_=zp[:, 2:2 + TB])
            elif j < 29:
                p.I("dve", "tensor_copy", out=kvl[:, j - 27, :], in_=zp[:, 2:2 + TB])
            else:
                p.I("dve", "tensor_copy", out=kp[:, :, :], in_=zp[0:64, 0:2 * TB].rearrange("p (a b) -> p a b", a=2))
        p.dma(sgate[:, t0:t0 + TB].rearrange("(c p) n -> p c n", p=128), sg[:])
        if "sgu" in stages:
            for sb_ in range(2):
                tok = slice(2 + 128 * sb_, 2 + 128 * (sb_ + 1))
                for k in range(16):
                    p.mm(TMp[:, 0:256], h[:, k, tok], wtm[:, k, 0:256], k == 0, k == 15)
                p.I("act", "activation", out=gv[:], in_=TMp[:, 0:256], func=AF.Gelu_apprx_tanh)
                p.I("dve", "tensor_reduce", out=st1[:, 0:2], in_=gv[:].rearrange("p (g c) -> p g c", g=2), axis=AX.X, op=ALU.add)
                p.I("dve", "tensor_scalar", out=st1[:, 2:4], in0=st1[:, 0:2], scalar1=-1.0 / 128, scalar2=None, op0=ALU.mult)
                for g in range(2):
                    gsl = slice(g * 128, (g + 1) * 128)
                    p.I("act", "activation", out=junk[:], in_=gv[:, gsl], func=AF.Square, bias=st1[:, 2 + g:3 + g], accum_out=st1[:, 4 + g:5 + g])
                    p.I("dve", "tensor_scalar", out=cen[:, gsl], in0=gv[:, gsl], scalar1=st1[:, 2 + g:3 + g], scalar2=None, op0=ALU.add)
                p.I("act", "activation", out=st1[:, 6:8], in_=st1[:, 4:6], func=AF.Sqrt, scale=1.0 / 128, bias=epsb[:])
                p.I("dve", "reciprocal", out=st1[:, 6:8], in_=st1[:, 6:8])
                for g in range(2):
                    gsl = slice(g * 128, (g + 1) * 128)
                    p.I("dve", "scalar_tensor_tensor", out=vn[:, gsl], in0=cen[:, gsl], scalar=st1[:, 6 + g:7 + g], in1=sgg[:, gsl], op0=ALU.mult, op1=ALU.mult)
                    msl = slice(256 + g * 128, 256 + (g + 1) * 128)
                    p.mm(TMp[:, msl], vn[:, gsl], wsT[:, g, :], True, False)
                    p.mm(TMp[:, msl], ones_bf[0:1, :], bsb[0:1, g, :], False, True)
                    p.I("dve", "tensor_tensor", out=oa[:, g, 128 * sb_:128 * (sb_ + 1)], in0=uT[:, g, 128 * sb_:128 * (sb_ + 1)], in1=TMp[:, msl], op=ALU.mult)
            p.dma(outA[:, t0:t0 + TB].rearrange("(c p) n -> p c n", p=128), oa[:])

        if "dn" in stages:
            for i in range(5):
                for j in range(0 if "noconv" in os.environ.get("MIXDBG", "") else 18):
                    if i == 0:
                        p.I("dve", "tensor_scalar", out=qkv[:, j, :], in0=zq[:, j, 0:TB], scalar1=cw[:, j, 0:1], scalar2=None, op0=ALU.mult)
                    else:
                        p.I("dve", "scalar_tensor_tensor", out=qkv[:, j, :], in0=zq[:, j, i:i + TB], scalar=cw[:, j, i:i + 1], in1=qkv[:, j, :], op0=ALU.mult, op1=ALU.add)
            pass
            DBG = os.environ.get("MIXDBG", "")
            for a3 in range(0 if "nosilu" in DBG else 3):
                p.I("act", "activation", out=qkv[:, 6 * a3:6 * a3 + 6, :], in_=qkv[:, 6 * a3:6 * a3 + 6, :], func=AF.Silu)
            for j2 in range(0 if "nonorm" in DBG else 6):
                p.I("act", "activation", out=sqq[:], in_=qkv[:, 2 * j2:2 * j2 + 2, :], func=AF.Square)
                wp = nxt()
                for jj in range(2):
                    p.mm(wp[:, jj * TB:(jj + 1) * TB], ones_bf[:], sqq[:, jj, :], True, True)
                p.I("act", "activation", out=rinv[:], in_=wp[:, :].rearrange("p (a b) -> p a b", a=2), func=AF.Sqrt, scale=1.0, bias=epsb[:])
                p.I("dve", "reciprocal", out=rinv[:], in_=rinv[:])
                if j2 < 3:
                    p.I("dve", "scalar_tensor_tensor", out=qkv[:, 2 * j2:2 * j2 + 2, :], in0=qkv[:, 2 * j2:2 * j2 + 2, :], scalar=128.0 ** -0.5, in1=rinv[:], op0=ALU.mult, op1=ALU.mult)
                else:
                    p.I("dve", "tensor_tensor", out=qkv[:, 2 * j2:2 * j2 + 2, :], in0=qkv[:, 2 * j2:2 * j2 + 2, :], in1=rinv[:], op=ALU.mult)
            for cc in range(4 if dnl >= 2 else 0):
                tok = slice(2 + 64 * cc, 2 + 64 * (cc + 1))
                for k in range(16):
                    p.mm(BC[0:64, 264 + cc * 12:264 + (cc + 1) * 12], h[:, k, tok], wtm[:, k, 256:268], k == 0, k == 15)
            if dnl >= 2:
                p.I("dve", "tensor_copy", out=ab4[:], in_=BC[0:64, 264:312].rearrange("p (a b) -> p a b", a=4))
                p.I("dve", "tensor_tensor", out=g4[:], in0=ab4[:, :, 0:6], in1=dtb[:], op=ALU.add)
                p.I("act", "activation", out=g4[:], in_=g4[:], func=AF.Exp)
                p.I("act", "activation", out=g4[:], in_=g4[:], func=AF.Ln, bias=1.0, scale=1.0)
                p.I("dve", "tensor_tensor", out=g4[:], in0=g4[:], in1=nea[:], op=ALU.mult)
                p.I("act", "activation", out=beta4[:], in_=ab4[:, :, 6:12], func=AF.Sigmoid)
            for cc in range(4 if dnl >= 3 else 0):
                tk = slice(cc * 64, (cc + 1) * 64)
                for hg in range(2):
                    d = DS[hg]
                    hs3 = [3 * hg + i for i in range(3)]
                    g3 = g4[:, cc, 3 * hg:3 * hg + 3]
                    b3 = beta4[:, cc, 3 * hg:3 * hg + 3]
                    wa = nxt()
                    for hh, hd in enumerate(hs3):
                        p.tr(wa[0:64, hh * 128:(hh + 1) * 128], qkv[:, 6 + hd, tk], ident[:])
                    p.I("act", "activation", out=d["ktm"][:], in_=wa[0:64, 0:384].rearrange("p (a b) -> p a b", a=3), func=AF.Identity)
                    wb = nxt()
                    for hh, hd in enumerate(hs3):
                        p.tr(wb[0:64, hh * 128:(hh + 1) * 128], qkv[:, 12 + hd, tk], ident[:])
                    p.I("act", "activation", out=d["vtm"][:], in_=wb[0:64, 0:384].rearrange("p (a b) -> p a b", a=3), func=AF.Identity)
                    if dnl < 4:
                        continue
                    L4 = int(os.environ.get("L4", "9"))
                    for hh in range(3):
                        p.I("dve", "tensor_scalar", out=d["Pm"][:, hh, :], in0=tri[:], scalar1=g3[:, hh:hh + 1], scalar2=None, op0=ALU.mult)
                    wc = nxt()
                    p.mm(wc[:, 0:192], ones_f[0:64, :], d["Pm"][:].rearrange("p a b -> p (a b)"), True, True)
                    if L4 >= 2:
                        p.mm(wc[0:64, 192:195], tri[:], g3, True, True)
                        p.I("dve", "tensor_copy", out=d["gcol"][:], in_=wc[0:64, 192:195])
                    if L4 >= 3:
                        p.I("act", "activation", out=d["EGB"][:], in_=wc[:, 0:192].rearrange("p (a b) -> p a b", a=3), func=(AF.Identity if os.environ.get("EGBID") else AF.Exp))
                    if L4 >= 4:
                        for hh in range(3):
                            p.I("dve", "tensor_scalar", out=d["D1s"][:, hh, :], in0=wc[0:64, hh * 64:(hh + 1) * 64], scalar1=d["gcol"][:, hh:hh + 1], scalar2=0.0, op0=ALU.subtract, op1=ALU.max)
                            p.I("dve", "tensor_scalar", out=d["D2"][:, hh, :], in0=wc[0:64, hh * 64:(hh + 1) * 64], scalar1=d["gcol"][:, hh:hh + 1], scalar2=0.0, op0=ALU.subtract, op1=ALU.min)
                    if L4 >= 5:
                        p.I("dve", "tensor_tensor", out=d["dl"][:], in0=wc[0:64, 0:192].rearrange("p (a b) -> p a b", a=3)[:, :, 63], in1=d["gcol"][:], op=ALU.subtract)
                    if L4 >= 6:
                        p.I("act", "activation", out=d["D1s"][:], in_=d["D1s"][:], func=AF.Exp, scale=-1.0)
                        p.I("act", "activation", out=d["D2"][:], in_=d["D2"][:], func=AF.Exp)
                        p.I("act", "activation", out=d["egc"][:], in_=d["gcol"][:], func=AF.Exp)
                        p.I("act", "activation", out=d["ekl"][:], in_=d["dl"][:], func=AF.Exp)
                    if L4 >= 7:
                        p.I("pool", "tensor_tensor", out=d["D1s"][:], in0=d["D1s"][:], in1=m_lows[:], op=ALU.mult)
                        p.I("pool", "tensor_tensor", out=d["D2"][:], in0=d["D2"][:], in1=m_up[:], op=ALU.mult)
                    if L4 >= 8:
                        p.I("dve", "tensor_tensor", out=d["be"][:], in0=b3, in1=d["egc"][:], op=ALU.mult)
                        p.I("dve", "tensor_scalar", out=d["nbeta"][:], in0=b3, scalar1=-1.0, scalar2=None, op0=ALU.mult)
                    if dnl < 5:
                        continue
                    wd = nxt()
                    for hh, hd in enumerate(hs3):
                        p.mm(wd[0:64, hh * 64:(hh + 1) * 64], qkv[:, 6 + hd, tk], qkv[:, 6 + hd, tk], True, True)
                    for hh, hd in enumerate(hs3):
                        p.mm(wd[0:64, 192 + hh * 64:192 + (hh + 1) * 64], qkv[:, 6 + hd, tk], qkv[:, hd, tk], True, True)
                    for hh in range(3):
                        p.I("dve", "scalar_tensor_tensor", out=d["Msb"][:, hh, :], in0=wd[0:64, hh * 64:(hh + 1) * 64], scalar=d["nbeta"][:, hh:hh + 1], in1=d["D1s"][:, hh, :], op0=ALU.mult, op1=ALU.mult)
                    p.I("dve", "tensor_tensor", out=d["qkT"][:], in0=wd[0:64, 192:384].rearrange("p (a b) -> p a b", a=3), in1=d["D2"][:], op=ALU.mult)
                    if dnl < 6:
                        continue
                    PP = d["PP"]; TT = d["TT"]
                    we = nxt()
                    for hh in range(3):
                        p.tr(we[0:64, hh * 64:(hh + 1) * 64], d["Msb"][:, hh, :], ident[0:64, 0:64])
                    p.I("act", "activation", out=PP[0][:, 1, :, :], in_=we[0:64, 0:192].rearrange("p (a b) -> p a b", a=3), func=AF.Identity)
                    p.I("pool", "tensor_copy", out=PP[0][:, 0, :, :], in_=d["Msb"][:])
                    p.I("pool", "tensor_tensor", out=TT[0][:], in0=PP[0][:, 1, :, :], in1=id3[:], op=ALU.add)
                    for lev in range(1, 6):
                        Po = PP[(lev - 1) % 2]; Pn = PP[lev % 2]
                        To = TT[(lev - 1) % 2]; Tn = TT[lev % 2]
                        wx = nxt()
                        for hh in range(3):
                            p.mm(wx[0:64, hh * 64:(hh + 1) * 64], Po[:, 1, hh, :], Po[:, 0, hh, :], True, True)
                        if lev < 5:
                            for hh in range(3):
                                p.mm(wx[0:64, 192 + hh * 64:192 + (hh + 1) * 64], Po[:, 0, hh, :], Po[:, 1, hh, :], True, True)
                            p.I("act", "activation", out=Pn[:].rearrange("p a b c -> p (a b c)"), in_=wx[0:64, 0:384], func=AF.Identity)
                        else:
                            p.I("act", "activation", out=Pn[:, 0, :, :].rearrange("p b c -> p (b c)"), in_=wx[0:64, 0:192], func=AF.Identity)
                        wy = nxt()
                        for hh in range(3):
                            p.mm(wy[0:64, hh * 64:(hh + 1) * 64], Pn[:, 0, hh, :], To[:, hh, :], True, True)
                        p.I("dve", "tensor_tensor", out=Tn[:].rearrange("p a b -> p (a b)"), in0=To[:].rearrange("p a b -> p (a b)"), in1=wy[0:64, 0:192], op=ALU.add)
                    TTf = TT[5 % 2]
                    if dnl < 7:
                        continue
                    for hh in range(3):
                        p.I("act", "activation", out=d["vb"][:, hh, :], in_=d["vtm"][:, hh, :], func=AF.Identity, scale=b3[:, hh:hh + 1])
                        p.I("act", "activation", out=d["kbg"][:, hh, :], in_=d["ktm"][:, hh, :], func=AF.Identity, scale=d["be"][:, hh:hh + 1])
                        p.I("dve", "tensor_scalar", out=d["kg"][:, hh, :], in0=d["ktm"][:, hh, :], scalar1=d["ekl"][:, hh:hh + 1], scalar2=None, op0=ALU.mult)
                    wu = nxt()
                    for hh in range(3):
                        p.mm(wu[0:64, hh * 128:(hh + 1) * 128], TTf[:, hh, :], d["vb"][:, hh, :], True, True)
                    p.I("act", "activation", out=d["u"][:].rearrange("p a b -> p (a b)"), in_=wu[0:64, 0:384], func=AF.Identity)
                    ww = nxt()
                    for hh in range(3):
                        p.mm(ww[:, hh * 64:(hh + 1) * 64], d["kbg"][:, hh, :], TTf[:, hh, :], True, True)
                    p.I("dve", "tensor_copy", out=d["wT"][:].rearrange("p a b -> p (a b)"), in_=ww[:, 0:192])
                    p.I("dve", "tensor_tensor", out=d["qgT"][:], in0=qkv[:, 3 * hg:3 * hg + 3, tk], in1=d["EGB"][:], op=ALU.mult)
                    if dnl < 8:
                        continue
                    S_f = Sf[hg]; S_b = Sb[hg]
                    ws_ = nxt()
                    for hh in range(3):
                        p.mm(ws_[0:64, hh * 128:(hh + 1) * 128], d["wT"][:, hh, :], S_b[:, hh, :], True, True)
                    p.I("dve", "tensor_tensor", out=d["vnew"][:].rearrange("p a b -> p (a b)"), in0=d["u"][:].rearrange("p a b -> p (a b)"), in1=ws_[0:64, 0:384], op=ALU.subtract)
                    wo = nxt()
                    for hh in range(3):
                        p.mm(wo[:, hh * 64:(hh + 1) * 64], S_b[:, hh, :], d["qgT"][:, hh, :], True, False)
                        p.mm(wo[:, hh * 64:(hh + 1) * 64], d["vnew"][:, hh, :], d["qkT"][:, hh, :], False, True)
                    wt_ = nxt()
                    for hh in range(3):
                        p.mm(wt_[:, hh * 128:(hh + 1) * 128], d["kg"][:, hh, :], d["vnew"][:, hh, :], True, True)
                    p.I("act", "activation", out=osb[:, 3 * hg:3 * hg + 3, tk], in_=wo[:, 0:192].rearrange("p (a b) -> p a b", a=3), func=AF.Identity)
                    for hh in range(3):
                        p.I("dve", "scalar_tensor_tensor", out=S_f[:, hh, :], in0=S_f[:, hh, :], scalar=d["EGB"][:, hh, 63:64], in1=wt_[:, hh * 128:(hh + 1) * 128], op0=ALU.mult, op1=ALU.add)
                    p.I("act", "activation", out=S_b[:], in_=S_f[:], func=AF.Identity)
            for hg in range(2):
                p.dma(oT[384 * hg:384 * (hg + 1), t0:t0 + TB].rearrange("(c p) n -> p c n", p=128), osb[:, 3 * hg:3 * hg + 3, :])
            if "dbgq" in stages and n == 1:
                for j in range(18):
                    p.dma(dbg[:, j, 0:TB], qkv[:, j, :])
                if dnl >= 2:
                    p.dma(dbg[0:64, 18, 0:24], g4[:].rearrange("p a b -> p (a b)"))
                    p.dma(dbg[0:64, 19, 0:24], beta4[:].rearrange("p a b -> p (a b)"))

        if "mla" in stages:
            p.dma(cosb[:], cosT_d[:, t0:t0 + TB])
            p.dma(sinb[:], sinT_d[:, t0:t0 + TB])
            p.I("act", "activation", out=sq4[:], in_=ql[:], func=AF.Square)
            wp = nxt()
            for c in range(4):
                p.mm(wp[:, 0:TB], ones_bf[:], sq4[:, c, :], c == 0, c == 3)
            p.I("act", "activation", out=rq[:], in_=wp[:, 0:TB], func=AF.Sqrt, scale=1.0 / 512, bias=epsb[:])
            p.I("dve", "reciprocal", out=rq[:], in_=rq[:])
            for c in range(4):
                p.I("dve", "scalar_tensor_tensor", out=cqT[:, c, :], in0=ql[:, c, :], scalar=qng[:, c:c + 1], in1=rq[:], op0=ALU.mult, op1=ALU.mult)
            p.I("act", "activation", out=sq4[:, 0:2, :], in_=kvl[:], func=AF.Square)
            wp = nxt()
            for c in range(2):
                p.mm(wp[:, 0:TB], ones_bf[:], sq4[:, c, :], c == 0, c == 1)
            p.I("act", "activation", out=rq[:], in_=wp[:, 0:TB], func=AF.Sqrt, scale=1.0 / 256, bias=epsb[:])
            p.I("dve", "reciprocal", out=rq[:], in_=rq[:])
            for c in range(2):
                p.I("dve", "scalar_tensor_tensor", out=ckvT[:, c, :], in0=kvl[:, c, :], scalar=kvng[:, c:c + 1], in1=rq[:], op0=ALU.mult, op1=ALU.mult)
            for hh in range(3):
                wq = nxt()
                for k in range(4):
                    p.mm(wq[:, 0:TB], wuq[:, k, hh * 256:hh * 256 + 128], cqT[:, k, :], k == 0, k == 3)
                p.I("act", "activation", out=qn_sb[:, hh, :], in_=wq[:, 0:TB], func=AF.Identity)
                wq2 = nxt()
                for half in range(2):
                    for k in range(4):
                        p.mm(wq2[0:64, half * TB:(half + 1) * TB], wuq[:, k, hh * 256 + 128 + half * 64:hh * 256 + 192 + half * 64], cqT[:, k, :], k == 0, k == 3)
                p.I("dve", "tensor_tensor", out=t1[:], in0=wq2[0:64, 0:TB], in1=cosb[:], op=ALU.mult)
                p.I("dve", "tensor_tensor", out=t2[:], in0=wq2[0:64, TB:2 * TB], in1=sinb[:], op=ALU.mult)
                p.I("dve", "tensor_tensor", out=qp_sb[:, hh, :], in0=t1[:], in1=t2[:], op=ALU.add)
            for hh in range(3):
                wk = nxt()
                for k in range(2):
                    p.mm(wk[:, 0:TB], wukv[:, k, hh * 128:(hh + 1) * 128], ckvT[:, k, :], k == 0, k == 1)
                p.I("act", "activation", out=kn_sb[:, hh, :], in_=wk[:, 0:TB], func=AF.Identity)
            p.I("dve", "tensor_tensor", out=t1[:], in0=kp[:, 0, :], in1=cosb[:], op=ALU.mult)
            p.I("dve", "tensor_tensor", out=t2[:], in0=kp[:, 1, :], in1=sinb[:], op=ALU.mult)
            p.I("dve", "tensor_tensor", out=kp_sb[:], in0=t1[:], in1=t2[:], op=ALU.add)
            for sb_ in range(2):
                wv = nxt()
                for k in range(2):
                    p.mm(wv[:, 0:384], ckvT[:, k, 128 * sb_:128 * (sb_ + 1)], wukv[:, k, 384:768], k == 0, k == 1)
                p.I("act", "activation", out=v_sb[:, sb_, :], in_=wv[:, 0:384], func=AF.Identity)
            p.dma(Ksn[:, :, t0:t0 + TB], kn_sb[:])
            p.dma(Ksp[:, t0:t0 + TB], kp_sb[:])
            p.dma(Vsc[:, t0 // 128:t0 // 128 + 2, :], v_sb[:])
            p.dma(Qsn[:, :, t0:t0 + TB], qn_sb[:])
            p.dma(Qsp[:, :, t0:t0 + TB], qp_sb[:])

    es1.close()
    p.barrier()

    if "attn" in stages:
        es2 = ExitStack()

        def sb2(shape, dtype=F32):
            return p.sb(shape, dtype, es=es2)
        nkt = NKT if nblk == NBLK else (nblk * TB) // 128
        ntk = nkt * 128
        Kn = sb2([128, 3, NTOK], BF16)
        Kp = sb2([64, NTOK], BF16)
        Vt = sb2([128, NKT, 384], BF16)
        for hh in range(3):
            p.dma(Kn[:, hh, 0:ntk], Ksn[:, hh, 0:ntk])
        p.dma(Kp[:, 0:ntk], Ksp[:, 0:ntk])
        for a in range(0, nkt, 16):
            b = min(nkt, a + 16)
            p.dma(Vt[:, a:b, :], Vsc[:, a:b, :])
        Qn = [sb2([128, 3, QB], BF16) for _ in range(2)]
        Qp = [sb2([64, 3, QB], BF16) for _ in range(2)]
        ksq = sb2([128, QB], BF16)
        ksq2 = sb2([64, QB], BF16)
        kmx = sb2([128, 3, 32])
        kmax = sb2([128, 3])
        qmax = sb2([128, 1])
        nbias = sb2([128, 1])
        PT = [sb2([128, QB], BF16) for _ in range(3)]
        rrs = sb2([128, QB])
        oc = [sb2([128, QB]) for _ in range(2)]
        SP = [PS[1], PS[2]]
        OP = [PS[4], PS[5]]
        RP = [PS[6], PS[7]]
        BQ = PS[3]
        p.op("dve", lambda e: e.memset(kmx[:].ap, 0.0), [], [kmx])
        ngrp = (ntk + QB - 1) // QB
        for hh in range(3):
            for gi in range(ngrp):
                a = gi * QB
                wd_ = min(QB, ntk - a)
                p.I("act", "activation", out=ksq[:, 0:wd_], in_=Kn[:, hh, a:a + wd_], func=AF.Square)
                p.I("act", "activation", out=ksq2[:, 0:wd_], in_=Kp[:, a:a + wd_], func=AF.Square)
                p.mm(BQ[:, 0:wd_], ones_bf[:], ksq[:, 0:wd_], True, False)
                p.mm(BQ[:, 0:wd_], ones_bf[0:64, :], ksq2[:, 0:wd_], False, True)
                p.I("dve", "tensor_reduce", out=kmx[:, hh, gi:gi + 1], in_=BQ[:, 0:wd_], axis=AX.X, op=ALU.max)
            p.I("dve", "tensor_reduce", out=kmax[:, hh:hh + 1], in_=kmx[:, hh, :], axis=AX.X, op=ALU.max)
        qblocks = [(0, CTX, [0, 1])]
        nlat = ntk - CTX
        for a in range(0, nlat, QB):
            qblocks.append((CTX + a, min(QB, nlat - a), list(range(nkt))))
        for qi, (q0, nq, kts) in enumerate(qblocks):
            qn = Qn[qi % 2]; qp = Qp[qi % 2]
            p.dma(qn[:, :, 0:nq], Qsn[:, :, q0:q0 + nq])
            p.dma(qp[:, :, 0:nq], Qsp[:, :, q0:q0 + nq])
            for hh in range(3):
                p.I("act", "activation", out=ksq[:, 0:nq], in_=qn[:, hh, 0:nq], func=AF.Square)
                p.I("act", "activation", out=ksq2[:, 0:nq], in_=qp[:, hh, 0:nq], func=AF.Square)
                p.mm(BQ[:, 0:nq], ones_bf[:], ksq[:, 0:nq], True, False)
                p.mm(BQ[:, 0:nq], ones_bf[0:64, :], ksq2[:, 0:nq], False, True)
                p.I("dve", "tensor_reduce", out=qmax[:], in_=BQ[:, 0:nq], axis=AX.X, op=ALU.max)
                p.I("dve", "tensor_tensor", out=qmax[:], in0=qmax[:], in1=kmax[:, hh:hh + 1], op=ALU.mult)
                p.I("act", "activation", out=nbias[:], in_=qmax[:], func=AF.Sqrt)
                p.I("dve", "tensor_scalar", out=nbias[:], in0=nbias[:], scalar1=-ASCALE, scalar2=None, op0=ALU.mult)
                op_ = OP[hh % 2]; rp_ = RP[hh % 2]
                nk = len(kts)

                def smm(i):
                    kt = kts[i]
                    spb = SP[i % 2]
                    p.mm(spb[:, 0:nq], Kn[:, hh, kt * 128:(kt + 1) * 128], qn[:, hh, 0:nq], True, False)
                    p.mm(spb[:, 0:nq], Kp[:, kt * 128:(kt + 1) * 128], qp[:, hh, 0:nq], False, True)
                smm(0)
                for i in range(nk):
                    if i + 1 < nk:
                        smm(i + 1)
                    pt = PT[i % 3]
                    p.I("act", "activation", out=pt[:, 0:nq], in_=SP[i % 2][:, 0:nq], func=AF.Exp, scale=ASCALE, bias=nbias[:])
                    p.mm(op_[:, 0:nq], Vt[:, kts[i], hh * 128:(hh + 1) * 128], pt[:, 0:nq], i == 0, i == nk - 1)
                    p.mm(rp_[:, 0:nq], ones_bf[:], pt[:, 0:nq], i == 0, i == nk - 1)
                p.I("dve", "reciprocal", out=rrs[:, 0:nq], in_=rp_[:, 0:nq])
                o_ = oc[hh % 2]
                p.I("dve", "tensor_tensor", out=o_[:, 0:nq], in0=op_[:, 0:nq], in1=rrs[:, 0:nq], op=ALU.mult)
                p.dma(outC[hh * 128:(hh + 1) * 128, q0:q0 + nq], o_[:, 0:nq])
        es2.close()

    p.finish()
    return nc, p

import os

D = 2048
EPS = 1e-6
PT = 512


def post_blocks(nctx, nlat):
    blks = []
    if nctx:
        blks.append((0, nctx, 1))
    for a in range(0, nlat, PT):
        blks.append((nctx + a, min(PT, nlat - a), 0))
    return blks


def build_post(kind, nctx, nlat, final, nblk=None, nexp=8, nf_override=None):
    nc = bass.Bass("TRN2", target_bir_lowering=False)
    p = Prog(nc)
    NT = nctx + nlat
    NF = (5632 if kind == "ffn" else 7168) // 128
    if nf_override:
        NF = nf_override
    NE = 1 if kind == "ffn" else nexp
    route = kind == "route"
    if route:
        NE = 0
        NF = 1

    def din(name, shape):
        return nc.dram_tensor(name, list(shape), F32, kind="ExternalInput").ap()

    xT = din("xT", [D, NT])
    aT = din("aT", [512, NT])
    ofT = din("ofT", [768, NT])
    obT = din("obT", [768, NT])
    sgT = din("sgT", [768, NT])
    cT = din("cT", [768, NT])
    c2T = din("c2T", [D, 2])
    ada_w = din("ada_w", [D, 4 * D])
    ada_bT = din("ada_bT", [128, 64])
    n2g = din("n2g", [128, 16])
    dng_d = din("dng", [128, 1])
    fg_d = din("fg", [128, 16])
    w_out = din("w_out", [D, D])
    c_ones = din("c_ones", [128, 128])
    c_ident = din("c_ident", [128, 128])
    if route:
        rt_d = din("router", [D, 8])
        h2T_o = nc.dram_tensor("h2T", [D, NT], F32, kind="ExternalOutput").ap()
        G_o = nc.dram_tensor("G", [NT, 8], F32, kind="ExternalOutput").ap()
    elif kind == "ffn":
        wg_d = [din("wg", [D, NF * 128])]
        wu_d = [din("wu", [D, NF * 128])]
        wd_d = [din("wd", [NF * 128, D])]
    else:
        wg_d = [din(f"wg{e}", [D, NF * 128]) for e in range(NE)]
        wu_d = [din(f"wu{e}", [D, NF * 128]) for e in range(NE)]
        wd_d = [din(f"wd{e}", [NF * 128, D]) for e in range(NE)]
        rt_d = din("router", [D, 8])
    xoT = nc.dram_tensor("xoT", [D, NT], F32, kind="ExternalOutput").ap()

    ones_bf = p.sb([128, 128], BF16)
    p.dma(ones_bf[:], c_ones, q="pool")
    ones_f = p.sb([128, 128])
    p.dma(ones_f[:], c_ones)
    ident_f = p.sb([128, 128])
    p.dma(ident_f[:], c_ident)
    epsb = p.sb([128, 1])
    p.op("dve", lambda e: e.memset(epsb[:].ap, EPS), [], [epsb])
    PS = [p.ps([128, 512]) for _ in range(8)]
    BCp = PS[0]
    Yp = [PS[1], PS[2]]
    Gp = [PS[3], PS[4]]
    Up = [PS[5], PS[6]]
    LG = PS[7]

    x = p.sb([128, 16, PT])
    c2 = p.sb([128, 16, 2])
    p.dma(c2[:], c2T.rearrange("(c p) n -> p c n", p=128))
    sc2 = p.sb([128, 16, 2])
    p.I("act", "activation", out=sc2[:], in_=c2[:], func=AF.Silu)
    adab = p.sb([128, 64])
    p.dma(adab[:], ada_bT)
    n2 = p.sb([128, 16])
    p.dma(n2[:], n2g)
    dng = p.sb([128, 1])
    p.dma(dng[:], dng_d)
    fg = p.sb([128, 16])
    p.dma(fg[:], fg_d)
    modT = p.sb([128, 64, 2])
    awt = [x[:, 0:4, :].rearrange("p a (b c) -> p (a b) c", c=128), x[:, 4:8, :].rearrange("p a (b c) -> p (a b) c", c=128)]
    for j in range(64):
        w = awt[j % 2]
        p.dma(w, ada_w[:, j * 128:(j + 1) * 128].rearrange("(c p) n -> p c n", p=128))
        for k in range(16):
            p.mm(BCp[:, 2 * (j % 2):2 * (j % 2) + 2], w[:, k, :], sc2[:, k, :], k == 0, k == 15)
        p.I("dve", "tensor_scalar", out=modT[:, j, :], in0=BCp[:, 2 * (j % 2):2 * (j % 2) + 2], scalar1=adab[:, j:j + 1],
            scalar2=None, op0=ALU.add)
    gs2 = p.sb([128, 16, 2])
    for s in range(2):
        p.I("dve", "scalar_tensor_tensor", out=gs2[:, :, s], in0=modT[:, 32:48, s], scalar=1.0, in1=n2[:], op0=ALU.add, op1=ALU.mult)
    if kind in ("moe", "route"):
        rt = p.sb([128, 16, 8])
        p.dma(rt[:], rt_d.rearrange("(c p) n -> p c n", p=128))
    p.barrier()

    mix = p.sb([128, 16, PT], BF16)
    h2 = p.sb([128, 16, PT], BF16)
    hid = p.sb([128, NF, PT], BF16)
    ld = [p.sb([128, PT]) for _ in range(4)]
    o32 = p.sb([128, PT])
    sqb = [p.sb([128, PT], BF16) for _ in range(2)]
    rs = p.sb([128, PT])
    tf = [p.sb([128, PT]) for _ in range(2)]
    wt = [p.sb([128, 16, 128], BF16) for _ in range(4)]
    wdt = [p.sb([128, NF, 128], BF16) for _ in range(2)]
    sgt = [p.sb([128, PT]) for _ in range(2)]
    if kind in ("moe", "route"):
        h2f = [p.sb([128, PT]) for _ in range(2)]
        h2ff = p.sb([128, 16, PT]) if route else None
        lg = p.sb([128, 4, 8]); lg2 = p.sb([128, 4, 8])
        eq1 = p.sb([128, 4, 8]); eq2 = p.sb([128, 4, 8])
        m1 = p.sb([128, 4]); m2 = p.sb([128, 4]); dl = p.sb([128, 4]); w1 = p.sb([128, 4]); w2 = p.sb([128, 4])
        gate = p.sb([128, 4, 8])
        gl = [p.sb([128, 128]) for _ in range(2)]
        gbc = p.sb([128, max(NE, 1), PT], BF16)
        sg2 = [p.sb([128, PT]) for _ in range(2)]
    wcnt = [0]
    ldc = [0]

    def nld():
        t = ld[ldc[0] % 4]
        ldc[0] += 1
        return t

    def nwt():
        t = wt[wcnt[0] % 4]
        wcnt[0] += 1
        return t

    blks = post_blocks(nctx, nlat)
    if nblk:
        blks = blks[:nblk]
    for (t0, T, s) in blks:
        for c in range(16):
            p.dma(x[:, c, 0:T], xT[c * 128:(c + 1) * 128, t0:t0 + T])
        for hd in range(6):
            a_ = nld(); b_ = nld(); g_ = nld()
            p.dma(a_[:, 0:T], ofT[hd * 128:(hd + 1) * 128, t0:t0 + T])
            p.dma(b_[:, 0:T], obT[hd * 128:(hd + 1) * 128, t0:t0 + T])
            p.dma(g_[:, 0:T], sgT[hd * 128:(hd + 1) * 128, t0:t0 + T])
            p.I("dve", "tensor_tensor", out=o32[:, 0:T], in0=a_[:, 0:T], in1=b_[:, 0:T], op=ALU.add)
            sq = sqb[hd % 2]
            p.I("act", "activation", out=sq[:, 0:T], in_=o32[:, 0:T], func=AF.Square)
            p.mm(BCp[:, 0:T], ones_bf[:], sq[:, 0:T], True, True)
            p.I("act", "activation", out=rs[:, 0:T], in_=BCp[:, 0:T], func=AF.Sqrt, scale=1.0 / 128, bias=epsb[:])
            p.I("dve", "reciprocal", out=rs[:, 0:T], in_=rs[:, 0:T])
            p.I("dve", "scalar_tensor_tensor", out=o32[:, 0:T], in0=o32[:, 0:T], scalar=dng[:, 0:1], in1=rs[:, 0:T], op0=ALU.mult, op1=ALU.mult)
            p.I("dve", "tensor_tensor", out=mix[:, 4 + hd, 0:T], in0=o32[:, 0:T], in1=g_[:, 0:T], op=ALU.mult)
        for g in range(4):
            a_ = nld()
            p.dma(a_[:, 0:T], aT[g * 128:(g + 1) * 128, t0:t0 + T])
            p.I("act", "activation", out=mix[:, g, 0:T], in_=a_[:, 0:T], func=AF.Identity)
        for hd in range(6):
            a_ = nld()
            p.dma(a_[:, 0:T], cT[hd * 128:(hd + 1) * 128, t0:t0 + T])
            p.I("act", "activation", out=mix[:, 10 + hd, 0:T], in_=a_[:, 0:T], func=AF.Identity)
        for m in range(16):
            w = nwt()
            p.dma(w[:], w_out[:, m * 128:(m + 1) * 128].rearrange("(c p) n -> p c n", p=128), q="pool")
            yp = Yp[m % 2]
            for k in range(16):
                p.mm(yp[:, 0:T], w[:, k, :], mix[:, k, 0:T], k == 0, k == 15)
            p.I("dve", "scalar_tensor_tensor", out=x[:, m, 0:T], in0=yp[:, 0:T], scalar=modT[:, m, s:s + 1], in1=x[:, m, 0:T], op0=ALU.mult, op1=ALU.add)
        for c in range(16):
            sq = sqb[c % 2]
            p.I("act", "activation", out=sq[:, 0:T], in_=x[:, c, 0:T], func=AF.Square)
            p.mm(BCp[:, 0:T], ones_bf[:], sq[:, 0:T], c == 0, c == 15)
        p.I("act", "activation", out=rs[:, 0:T], in_=BCp[:, 0:T], func=AF.Sqrt, scale=1.0 / D, bias=epsb[:])
        p.I("dve", "reciprocal", out=rs[:, 0:T], in_=rs[:, 0:T])
        nsb = T // 128
        for c in range(16):
            t_ = tf[c % 2]
            p.I("dve", "tensor_tensor", out=t_[:, 0:T], in0=x[:, c, 0:T], in1=rs[:, 0:T], op=ALU.mult)
            if kind == "ffn":
                p.I("act", "activation", out=h2[:, c, 0:T], in_=t_[:, 0:T], func=AF.Identity, scale=gs2[:, c, s:s + 1], bias=modT[:, 16 + c, s:s + 1])
            else:
                if route:
                    p.I("act", "activation", out=h2ff[:, c, 0:T], in_=t_[:, 0:T], func=AF.Identity, scale=gs2[:, c, s:s + 1], bias=modT[:, 16 + c, s:s + 1])
                    p.dma(h2T_o[c * 128:(c + 1) * 128, t0:t0 + T], h2ff[:, c, 0:T])
                else:
                    raise NotImplementedError("dense-masked moe path disabled")
        if route:
            for sbk in range(nsb):
                for c in range(16):
                    p.mm(LG[:, sbk * 8:(sbk + 1) * 8], h2ff[:, c, sbk * 128:(sbk + 1) * 128], rt[:, c, :], c == 0, c == 15)
        if kind in ("moe", "route"):
            p.I("dve", "tensor_copy", out=lg[:, 0:nsb, :], in_=LG[:, 0:nsb * 8].rearrange("p (a b) -> p a b", b=8))
            p.I("dve", "tensor_reduce", out=m1[:, 0:nsb], in_=lg[:, 0:nsb, :], axis=AX.X, op=ALU.max)
            for sbk in range(nsb):
                p.I("dve", "tensor_scalar", out=eq1[:, sbk, :], in0=lg[:, sbk, :], scalar1=m1[:, sbk:sbk + 1], scalar2=None, op0=ALU.is_equal)
            p.I("dve", "scalar_tensor_tensor", out=lg2[:, 0:nsb, :], in0=eq1[:, 0:nsb, :], scalar=-1e30, in1=lg[:, 0:nsb, :], op0=ALU.mult, op1=ALU.add)
            p.I("dve", "tensor_reduce", out=m2[:, 0:nsb], in_=lg2[:, 0:nsb, :], axis=AX.X, op=ALU.max)
            for sbk in range(nsb):
                p.I("dve", "tensor_scalar", out=eq2[:, sbk, :], in0=lg2[:, sbk, :], scalar1=m2[:, sbk:sbk + 1], scalar2=None, op0=ALU.is_equal)
            p.I("dve", "tensor_tensor", out=dl[:, 0:nsb], in0=m2[:, 0:nsb], in1=m1[:, 0:nsb], op=ALU.subtract)
            p.I("act", "activation", out=w1[:, 0:nsb], in_=dl[:, 0:nsb], func=AF.Sigmoid, scale=-1.0)
            p.I("act", "activation", out=w2[:, 0:nsb], in_=dl[:, 0:nsb], func=AF.Sigmoid)
            for sbk in range(nsb):
                p.I("dve", "tensor_scalar", out=eq1[:, sbk, :], in0=eq1[:, sbk, :], scalar1=w1[:, sbk:sbk + 1], scalar2=None, op0=ALU.mult)
                p.I("dve", "scalar_tensor_tensor", out=gate[:, sbk, :], in0=eq2[:, sbk, :], scalar=w2[:, sbk:sbk + 1], in1=eq1[:, sbk, :], op0=ALU.mult, op1=ALU.add)
            if route:
                p.dma(G_o[t0:t0 + T, :].rearrange("(a p) e -> p a e", p=128), gate[:, 0:nsb, :])
            for e in range(NE):
                for sbk in range(nsb):
                    g_ = gl[(e * nsb + sbk) % 2]
                    p.I("dve", "tensor_scalar", out=g_[:], in0=ones_f[:], scalar1=gate[:, sbk, e:e + 1], scalar2=None, op0=ALU.mult)
                    p.mm(LG[:, sbk * 128:(sbk + 1) * 128], g_[:], ident_f[:], True, True)
                p.I("act", "activation", out=gbc[:, e, 0:T], in_=LG[:, 0:T], func=AF.Identity)
        for e in range(NE):
            for f in range(NF):
                wg = nwt()
                p.dma(wg[:], wg_d[e][:, f * 128:(f + 1) * 128].rearrange("(c p) n -> p c n", p=128), q="pool")
                wu = nwt()
                p.dma(wu[:], wu_d[e][:, f * 128:(f + 1) * 128].rearrange("(c p) n -> p c n", p=128), q="pool")
                gp = Gp[f % 2]; up = Up[f % 2]
                for k in range(16):
                    p.mm(gp[:, 0:T], wg[:, k, :], h2[:, k, 0:T], k == 0, k == 15)
                for k in range(16):
                    p.mm(up[:, 0:T], wu[:, k, :], h2[:, k, 0:T], k == 0, k == 15)
                st = sgt[f % 2]
                p.I("act", "activation", out=st[:, 0:T], in_=gp[:, 0:T], func=AF.Silu)
                if kind == "moe":
                    s2 = sg2[f % 2]
                    p.I("pool", "tensor_tensor", out=s2[:, 0:T], in0=st[:, 0:T], in1=gbc[:, e, 0:T], op=ALU.mult)
                    p.I("dve", "tensor_tensor", out=hid[:, f, 0:T], in0=s2[:, 0:T], in1=up[:, 0:T], op=ALU.mult)
                else:
                    p.I("dve", "tensor_tensor", out=hid[:, f, 0:T], in0=st[:, 0:T], in1=up[:, 0:T], op=ALU.mult)
            for m in range(16):
                wd = wdt[m % 2]
                nsp = 4
                per = NF // nsp
                for q4 in range(nsp):
                    fa = q4 * per
                    fb = NF if q4 == nsp - 1 else (q4 + 1) * per
                    p.dma(wd[:, fa:fb, :], wd_d[e][fa * 128:fb * 128, m * 128:(m + 1) * 128].rearrange("(c p) n -> p c n", p=128), q="pool")
                yp = Yp[m % 2]
                for f in range(NF):
                    p.mm(yp[:, 0:T], wd[:, f, :], hid[:, f, 0:T], f == 0, f == NF - 1)
                p.I("dve", "scalar_tensor_tensor", out=x[:, m, 0:T], in0=yp[:, 0:T], scalar=modT[:, 48 + m, s:s + 1], in1=x[:, m, 0:T], op0=ALU.mult, op1=ALU.add)
        if final:
            for c in range(16):
                sq = sqb[c % 2]
                p.I("act", "activation", out=sq[:, 0:T], in_=x[:, c, 0:T], func=AF.Square)
                p.mm(BCp[:, 0:T], ones_bf[:], sq[:, 0:T], c == 0, c == 15)
            p.I("act", "activation", out=rs[:, 0:T], in_=BCp[:, 0:T], func=AF.Sqrt, scale=1.0 / D, bias=epsb[:])
            p.I("dve", "reciprocal", out=rs[:, 0:T], in_=rs[:, 0:T])
            for c in range(16):
                p.I("dve", "scalar_tensor_tensor", out=x[:, c, 0:T], in0=x[:, c, 0:T], scalar=fg[:, c:c + 1], in1=rs[:, 0:T], op0=ALU.mult, op1=ALU.mult)
        for c in range(16):
            p.dma(xoT[c * 128:(c + 1) * 128, t0:t0 + T], x[:, c, 0:T])
    p.finish()
    return nc, p


def build_expert(C, NF=56):
    nc = bass.Bass("TRN2", target_bir_lowering=False)
    p = Prog(nc)

    def din(name, shape):
        return nc.dram_tensor(name, list(shape), F32, kind="ExternalInput").ap()
    hT = din("hT", [D, C])
    gwb_d = din("gwb", [128, C])
    wg_d = din("wg", [D, NF * 128])
    wu_d = din("wu", [D, NF * 128])
    wd_d = din("wd", [NF * 128, D])
    yT = nc.dram_tensor("yT", [D, C], F32, kind="ExternalOutput").ap()
    PS = [p.ps([128, 512]) for _ in range(8)]
    Yp = [PS[1], PS[2]]
    Gp = [PS[3], PS[4]]
    Up = [PS[5], PS[6]]
    h2 = p.sb([128, 16, PT], BF16)
    hid = p.sb([128, NF, PT], BF16)
    ld = [p.sb([128, PT]) for _ in range(3)]
    gwb = p.sb([128, PT])
    wt = [p.sb([128, 16, 128], BF16) for _ in range(4)]
    wdt = [p.sb([128, NF, 128], BF16) for _ in range(2)]
    sgt = [p.sb([128, PT]) for _ in range(2)]
    sg2 = [p.sb([128, PT]) for _ in range(2)]
    yo = [p.sb([128, PT]) for _ in range(2)]
    wcnt = 0
    for t0 in range(0, C, PT):
        T = min(PT, C - t0)
        for c in range(16):
            a_ = ld[c % 3]
            p.dma(a_[:, 0:T], hT[c * 128:(c + 1) * 128, t0:t0 + T])
            p.I("act", "activation", out=h2[:, c, 0:T], in_=a_[:, 0:T], func=AF.Identity)
        p.dma(gwb[:, 0:T], gwb_d[:, t0:t0 + T])
        for f in range(NF):
            wg = wt[wcnt % 4]; wcnt += 1
            p.dma(wg[:], wg_d[:, f * 128:(f + 1) * 128].rearrange("(c p) n -> p c n", p=128), q="pool")
            wu = wt[wcnt % 4]; wcnt += 1
            p.dma(wu[:], wu_d[:, f * 128:(f + 1) * 128].rearrange("(c p) n -> p c n", p=128), q="pool")
            gp = Gp[f % 2]; up = Up[f % 2]
            for k in range(16):
                p.mm(gp[:, 0:T], wg[:, k, :], h2[:, k, 0:T], k == 0, k == 15)
            for k in range(16):
                p.mm(up[:, 0:T], wu[:, k, :], h2[:, k, 0:T], k == 0, k == 15)
            st = sgt[f % 2]
            p.I("act", "activation", out=st[:, 0:T], in_=gp[:, 0:T], func=AF.Silu)
            s2 = sg2[f % 2]
            p.I("pool", "tensor_tensor", out=s2[:, 0:T], in0=st[:, 0:T], in1=gwb[:, 0:T], op=ALU.mult)
            p.I("dve", "tensor_tensor", out=hid[:, f, 0:T], in0=s2[:, 0:T], in1=up[:, 0:T], op=ALU.mult)
        for m in range(16):
            wd = wdt[m % 2]
            nsp = 4
            per = NF // nsp
            for q4 in range(nsp):
                fa = q4 * per
                fb = NF if q4 == nsp - 1 else (q4 + 1) * per
                p.dma(wd[:, fa:fb, :], wd_d[fa * 128:fb * 128, m * 128:(m + 1) * 128].rearrange("(c p) n -> p c n", p=128), q="pool")
            yp = Yp[m % 2]
            for f in range(NF):
                p.mm(yp[:, 0:T], wd[:, f, :], hid[:, f, 0:T], f == 0, f == NF - 1)
            y_ = yo[m % 2]
            p.I("act", "activation", out=y_[:, 0:T], in_=yp[:, 0:T], func=AF.Identity)
            p.dma(yT[m * 128:(m + 1) * 128, t0:t0 + T], y_[:, 0:T])
    p.finish()
    return nc, p


def build_combine(NT):
    nc = bass.Bass("TRN2", target_bir_lowering=False)
    p = Prog(nc)

    def din(name, shape):
        return nc.dram_tensor(name, list(shape), F32, kind="ExternalInput").ap()
    xT = din("xT", [D, NT])
    y1T = din("y1T", [D, NT])
    y2T = din("y2T", [D, NT])
    c2T = din("c2T", [D, 2])
    ada_w = din("ada_w", [D, D])
    ada_bT = din("ada_bT", [128, 16])
    fg_d = din("fg", [128, 16])
    c_ones = din("c_ones", [128, 128])
    oT = nc.dram_tensor("oT", [D, NT], F32, kind="ExternalOutput").ap()
    ones_bf = p.sb([128, 128], BF16)
    p.dma(ones_bf[:], c_ones, q="pool")
    epsb = p.sb([128, 1])
    p.op("dve", lambda e: e.memset(epsb[:].ap, EPS), [], [epsb])
    PS = [p.ps([128, 512]) for _ in range(2)]
    BCp = PS[0]
    x = p.sb([128, 16, PT])
    c2 = p.sb([128, 16, 2])
    p.dma(c2[:], c2T.rearrange("(c p) n -> p c n", p=128))
    sc2 = p.sb([128, 16, 2])
    p.I("act", "activation", out=sc2[:], in_=c2[:], func=AF.Silu)
    adab = p.sb([128, 16])
    p.dma(adab[:], ada_bT)
    fg = p.sb([128, 16])
    p.dma(fg[:], fg_d)
    modT = p.sb([128, 16, 2])
    awt = [x[:, 0:4, :].rearrange("p a (b c) -> p (a b) c", c=128), x[:, 4:8, :].rearrange("p a (b c) -> p (a b) c", c=128)]
    for j in range(16):
        w = awt[j % 2]
        p.dma(w, ada_w[:, j * 128:(j + 1) * 128].rearrange("(c p) n -> p c n", p=128))
        for k in range(16):
            p.mm(PS[1][:, 2 * (j % 2):2 * (j % 2) + 2], w[:, k, :], sc2[:, k, :], k == 0, k == 15)
        p.I("dve", "tensor_scalar", out=modT[:, j, :], in0=PS[1][:, 2 * (j % 2):2 * (j % 2) + 2], scalar1=adab[:, j:j + 1],
            scalar2=None, op0=ALU.add)
    p.barrier()
    ld = [p.sb([128, PT]) for _ in range(4)]
    ys = [p.sb([128, PT]) for _ in range(2)]
    sqb = [p.sb([128, PT], BF16) for _ in range(2)]
    rs = p.sb([128, PT])
    for t0 in range(0, NT, PT):
        T = min(PT, NT - t0)
        for c in range(16):
            a_ = ld[(2 * c) % 4]; b_ = ld[(2 * c + 1) % 4]
            p.dma(x[:, c, 0:T], xT[c * 128:(c + 1) * 128, t0:t0 + T])
            p.dma(a_[:, 0:T], y1T[c * 128:(c + 1) * 128, t0:t0 + T])
            p.dma(b_[:, 0:T], y2T[c * 128:(c + 1) * 128, t0:t0 + T])
            y_ = ys[c % 2]
            p.I("dve", "tensor_tensor", out=y_[:, 0:T], in0=a_[:, 0:T], in1=b_[:, 0:T], op=ALU.add)
            p.I("dve", "scalar_tensor_tensor", out=x[:, c, 0:T], in0=y_[:, 0:T], scalar=modT[:, c, 0:1], in1=x[:, c, 0:T], op0=ALU.mult, op1=ALU.add)
            sq = sqb[c % 2]
            p.I("act", "activation", out=sq[:, 0:T], in_=x[:, c, 0:T], func=AF.Square)
            p.mm(BCp[:, 0:T], ones_bf[:], sq[:, 0:T], c == 0, c == 15)
        p.I("act", "activation", out=rs[:, 0:T], in_=BCp[:, 0:T], func=AF.Sqrt, scale=1.0 / D, bias=epsb[:])
        p.I("dve", "reciprocal", out=rs[:, 0:T], in_=rs[:, 0:T])
        for c in range(16):
            p.I("dve", "scalar_tensor_tensor", out=x[:, c, 0:T], in0=x[:, c, 0:T], scalar=fg[:, c:c + 1], in1=rs[:, 0:T], op0=ALU.mult, op1=ALU.mult)
            p.dma(oT[c * 128:(c + 1) * 128, t0:t0 + T], x[:, c, 0:T])
    p.finish()
    return nc, p

def _unflip(M):
    return np.concatenate([M[:, :CTX][:, ::-1], M[:, CTX:][:, ::-1]], axis=1)


def mix_host2(inp, li, r, XT_b, CT_b):
    b, dr = r // 2, r % 2
    f32 = np.float32
    xs = XT_b[:, ::-1] if dr else XT_b
    cs = CT_b[:, ::-1] if dr else CT_b
    xT = np.zeros((D, NCOL), f32)
    xT[:, 2:2 + CTX] = cs
    xT[:, NB + 2:NB + 2 + SEQ] = xs
    dummy = np.zeros((1, SEQ, 1), f32)
    m = mix_host(inp, li, r, None, None, xT_ready=xT)
    return m


def _run(nc, in_maps):
    res = run_bass_kernel_spmd(nc, in_maps, core_ids=list(range(len(in_maps))))
    return res.results


def kernel(**inputs):
    inp = {k: np.asarray(v) for k, v in inputs.items()}
    f32 = np.float32
    NB_ = 4
    XT = [np.ascontiguousarray(inp["x"][b].T) for b in range(NB_)]
    CTs = [np.ascontiguousarray(inp["ctx"][b].T) for b in range(NB_)]
    consts = make_consts()
    nc_mix, _ = build_mix(("sgu", "dn", "mla", "attn"), NBLK)
    HL = SEQ // 2
    HC = CTX // 2
    out_full = np.zeros((NB_, SEQ, D), f32)
    for li in range(2):
        in_maps = [mix_host2(inp, li, r, XT[r // 2], CTs[r // 2]) for r in range(8)]
        res = _run(nc_mix, in_maps)
        del in_maps
        post_in = []
        for r in range(8):
            b, hf = r // 2, r % 2
            A, B = res[2 * b], res[2 * b + 1]
            if li == 0:
                cols = np.concatenate([np.arange(hf * HC, (hf + 1) * HC), CTX + np.arange(hf * HL, (hf + 1) * HL)])
                xin = np.concatenate([CTs[b][:, hf * HC:(hf + 1) * HC], XT[b][:, hf * HL:(hf + 1) * HL]], axis=1)
            else:
                cols = CTX + np.arange(hf * HL, (hf + 1) * HL)
                xin = XT[b][:, hf * HL:(hf + 1) * HL]
            m = {}
            m["xT"] = np.ascontiguousarray(xin)
            m["aT"] = np.ascontiguousarray(np.concatenate([A["outA"][:, cols], _unflip(B["outA"])[:, cols]], axis=0))
            m["ofT"] = np.ascontiguousarray(A["oT"][:, cols])
            m["obT"] = np.ascontiguousarray(_unflip(B["oT"])[:, cols])
            m["sgT"] = np.ascontiguousarray(np.concatenate([A["sgate"][:, cols], _unflip(B["sgate"])[:, cols]], axis=0))
            m["cT"] = np.ascontiguousarray(np.concatenate([A["outC"][:, cols], _unflip(B["outC"])[:, cols]], axis=0))
            m["c2T"] = np.ascontiguousarray(np.stack([inp["c"][b], inp["c_ctx"]], axis=1))
            m["ada_w"] = np.ascontiguousarray(inp["ada_w"][li][:, 2 * D:])
            m["ada_bT"] = np.ascontiguousarray(inp["ada_b"][li][2 * D:].reshape(64, 128).T)
            m["n2g"] = np.ascontiguousarray(inp["norm2_g"][li].reshape(16, 128).T)
            m["dng"] = np.ascontiguousarray(inp["dn_norm_g"][li].reshape(128, 1))
            m["fg"] = np.ascontiguousarray(inp["final_norm_g"].reshape(16, 128).T)
            m["w_out"] = np.ascontiguousarray(inp["w_out"][li])
            m["c_ones"] = consts["ones"]
            m["c_ident"] = consts["ident"]
            if li == 0:
                m["wg"] = np.ascontiguousarray(inp["ffn_w_gate"][0])
                m["wu"] = np.ascontiguousarray(inp["ffn_w_up"][0])
                m["wd"] = np.ascontiguousarray(inp["ffn_w_down"][0])
            else:
                m["router"] = np.ascontiguousarray(inp["moe_router"][0])
            post_in.append(m)
        del res
        if li == 0:
            nc_p, _ = build_post("ffn", HC, HL, False)
            pres = _run(nc_p, post_in)
            del post_in
            for r in range(8):
                b, hf = r // 2, r % 2
                xo = pres[r]["xoT"]
                CTs[b][:, hf * HC:(hf + 1) * HC] = xo[:, :HC]
                XT[b][:, hf * HL:(hf + 1) * HL] = xo[:, HC:]
            del pres
        else:
            nc_p, _ = build_post("route", 0, HL, False)
            pres = _run(nc_p, post_in)
            del post_in
            NTOK_ALL = 8 * HL
            Gall = np.concatenate([pres[r]["G"] for r in range(8)], axis=0)
            H2 = np.concatenate([pres[r]["h2T"] for r in range(8)], axis=1)
            xmid = [pres[r]["xoT"] for r in range(8)]
            del pres
            idx = [np.nonzero(Gall[:, e])[0] for e in range(8)]
            cmax = max(len(i) for i in idx)
            C = ((cmax + 511) // 512) * 512
            ex_in = []
            for e in range(8):
                n = len(idx[e])
                hT = np.zeros((D, C), f32)
                hT[:, :n] = H2[:, idx[e]]
                gwb = np.zeros((128, C), f32)
                gwb[:, :n] = np.broadcast_to(Gall[idx[e], e][None, :], (128, n))
                ex_in.append({"hT": hT, "gwb": gwb,
                              "wg": np.ascontiguousarray(inp["moe_w_gate"][0][e]),
                              "wu": np.ascontiguousarray(inp["moe_w_up"][0][e]),
                              "wd": np.ascontiguousarray(inp["moe_w_down"][0][e])})
            del H2
            nc_e, _ = build_expert(C)
            eres = _run(nc_e, ex_in)
            del ex_in
            Y1 = np.zeros((D, NTOK_ALL), f32)
            Y2 = np.zeros((D, NTOK_ALL), f32)
            seen = np.zeros(NTOK_ALL, bool)
            for e in range(8):
                n = len(idx[e])
                cols = idx[e]
                y = eres[e]["yT"][:, :n]
                first = ~seen[cols]
                Y1[:, cols[first]] = y[:, first]
                Y2[:, cols[~first]] = y[:, ~first]
                seen[cols] = True
            del eres
            cb_in = []
            for r in range(8):
                b = r // 2
                cb_in.append({"xT": xmid[r], "y1T": np.ascontiguousarray(Y1[:, r * HL:(r + 1) * HL]),
                              "y2T": np.ascontiguousarray(Y2[:, r * HL:(r + 1) * HL]),
                              "c2T": np.ascontiguousarray(np.stack([inp["c"][b], inp["c_ctx"]], axis=1)),
                              "ada_w": np.ascontiguousarray(inp["ada_w"][1][:, 5 * D:]),
                              "ada_bT": np.ascontiguousarray(inp["ada_b"][1][5 * D:].reshape(16, 128).T),
                              "fg": np.ascontiguousarray(inp["final_norm_g"].reshape(16, 128).T),
                              "c_ones": consts["ones"]})
            nc_c, _ = build_combine(HL)
            cres = _run(nc_c, cb_in)
            for r in range(8):
                b, hf = r // 2, r % 2
                out_full[b, hf * HL:(hf + 1) * HL, :] = cres[r]["oT"].T
    return out_full
```

```python
import numpy as np
from contextlib import ExitStack
import concourse.bass as bass
import concourse.mybir as mybir
from concourse.bass_utils import run_bass_kernel_spmd

F32 = mybir.dt.float32
BF16 = mybir.dt.bfloat16
ALU = mybir.AluOpType
AF = mybir.ActivationFunctionType
AX = mybir.AxisListType

import os as _os
SYNCMODE = _os.environ.get('SYNCMODE', 'all')
EPOCH = 30000
NDS = 6


class TAP:
    __slots__ = ("tile", "ap")

    def __init__(self, tile, ap):
        self.tile = tile
        self.ap = ap

    def __getitem__(self, idx):
        return TAP(self.tile, self.ap[idx])

    def rearrange(self, *a, **k):
        return TAP(self.tile, self.ap.rearrange(*a, **k))


class Tile:
    __slots__ = ("t", "w", "r", "name", "psum")

    def __init__(self, t, name, psum=False):
        self.t = t
        self.w = None
        self.r = []
        self.name = name
        self.psum = psum

    def __getitem__(self, idx):
        return TAP(self, self.t[idx])


class Rec:
    __slots__ = ("eng", "fn", "dma", "deps", "signal", "sigval", "idx", "dn", "raw")

    def __init__(self, eng, fn, dma):
        self.eng = eng
        self.fn = fn
        self.dma = dma
        self.deps = ()
        self.raw = ()
        self.signal = False
        self.sigval = 0
        self.idx = 0
        self.dn = 0


WRITE_KEYS = ("out", "accum_out")
PTR_KEYS = ("scalar", "scalar1", "scalar2", "scale", "bias")


class Prog:
    ENG = ["pe", "act", "dve", "pool", "sp"]

    def __init__(self, nc, same_eng_sync=True):
        self.nc = nc
        self.es = ExitStack()
        self.ins = {e: [] for e in self.ENG}
        self.ndma = {e: 0 for e in self.ENG}
        self.dmarecs = {e: [] for e in self.ENG}
        self.same_eng_sync = same_eng_sync
        self.uid = 0
        self.pending = {e: set() for e in self.ENG}

    def sb(self, shape, dtype=F32, name=None, es=None):
        self.uid += 1
        name = name or f"sb{self.uid}"
        t = (es or self.es).enter_context(self.nc.sbuf_tensor(name, list(shape), dtype))
        return Tile(t, name)

    def ps(self, shape, dtype=F32, name=None, es=None):
        self.uid += 1
        name = name or f"ps{self.uid}"
        t = (es or self.es).enter_context(self.nc.psum_tensor(name, list(shape), dtype))
        return Tile(t, name, psum=True)

    def dram(self, name, shape, dtype, kind="Internal"):
        t = self.nc.dram_tensor(name, list(shape), dtype, kind=kind)
        return Tile(t.ap(), name)

    def view(self, ap, name="v"):
        return Tile(ap, name)

    def op(self, eng, fn, reads=(), writes=(), dma=False, ptr_reads=()):
        rec = Rec(eng, fn, dma)
        lst = self.ins[eng]
        rec.idx = len(lst)
        deps = set()
        for r in reads:
            if r.w is not None:
                deps.add(r.w)
        rec.raw = set(deps) if SYNCMODE == 'all' else set(t.w for t in ptr_reads if t.w is not None)
        for r in reads:
            if r.psum and r.r:
                best = {}
                for rr in r.r:
                    if rr.eng != eng:
                        b = best.get(rr.eng)
                        if b is None or rr.idx > b.idx:
                            best[rr.eng] = rr
                deps.update(best.values())
        for w in writes:
            if w.w is not None:
                deps.add(w.w)
            if w.r:
                best = {}
                for rr in w.r:
                    if rr.dma:
                        deps.add(rr)
                    else:
                        b = best.get(rr.eng)
                        if b is None or rr.idx > b.idx:
                            best[rr.eng] = rr
                deps.update(best.values())
        if self.pending[eng]:
            deps.update(self.pending[eng])
            self.pending[eng] = set()
        deps.discard(rec)
        rec.deps = deps
        for r in reads:
            r.r.append(rec)
        for w in writes:
            w.w = rec
            w.r = []
        if dma:
            rec.dn = self.ndma[eng]
            self.ndma[eng] += 1
            self.dmarecs[eng].append(rec)
        lst.append(rec)
        return rec

    def I(self, eng, meth, *args, **kw):
        reads, writes, ptrs = [], [], []
        kw2 = {}
        for k, v in kw.items():
            if isinstance(v, TAP):
                tl = v.tile if isinstance(v.tile, tuple) else (v.tile,)
                if k in PTR_KEYS:
                    ptrs.extend(tl)
                if k in WRITE_KEYS:
                    writes.extend(tl)
                    if k == "accum_out":
                        reads.extend(tl)
                else:
                    reads.extend(tl)
                kw2[k] = v.ap
            else:
                kw2[k] = v
        return self.op(eng, lambda e: getattr(e, meth)(**kw2), reads, writes, ptr_reads=ptrs)

    def dma(self, out, in_, q="sp", **kw):
        reads, writes = [], []
        if isinstance(out, TAP):
            writes.extend(out.tile if isinstance(out.tile, tuple) else (out.tile,))
            out = out.ap
        if isinstance(in_, TAP):
            reads.extend(in_.tile if isinstance(in_.tile, tuple) else (in_.tile,))
            in_ = in_.ap
        return self.op(q, lambda e: e.dma_start(out=out, in_=in_, **kw), reads, writes, dma=True)

    def mm(self, out, lhsT, rhs, start=True, stop=True, extra_reads=()):
        reads = list(extra_reads)
        for t in (lhsT.tile, rhs.tile):
            reads.extend(t if isinstance(t, tuple) else (t,))
        o, l, r = out.ap, lhsT.ap, rhs.ap
        return self.op("pe", lambda e: e.matmul(o, l, r, start=start, stop=stop), reads, [out.tile])

    def tr(self, out, in_, ident):
        o, i, d = out.ap, in_.ap, ident.ap
        rd = list(in_.tile if isinstance(in_.tile, tuple) else (in_.tile,)) + [ident.tile]
        return self.op("pe", lambda e: e.transpose(out=o, in_=i, identity=d), rd, [out.tile])

    def barrier(self):
        last = set()
        for e in self.ENG:
            if self.ins[e]:
                last.add(self.ins[e][-1])
            for rec in self.dmarecs[e][-NDS:]:
                last.add(rec)
        for e in self.ENG:
            self.pending[e] = set(last)

    def _needs_wait(self, rec, d):
        if d.dma:
            return True
        if d.eng == rec.eng:
            if rec.dma:
                return True
            if rec.eng == "pe":
                return False
            return self.same_eng_sync and (SYNCMODE == 'all' or d in rec.raw)
        return True

    def finish(self):
        nc = self.nc
        for e in self.ENG:
            for rec in self.ins[e]:
                for d in rec.deps:
                    if (not d.dma) and self._needs_wait(rec, d):
                        d.signal = True
        nsig = {}
        for e in self.ENG:
            c = 0
            for rec in self.ins[e]:
                if rec.signal:
                    c += 1
                    rec.sigval = c
            nsig[e] = c
        self.nsig = nsig
        sems = {}
        for e in self.ENG:
            n_ep = (nsig[e] + EPOCH - 1) // EPOCH
            sems[e] = [self.es.enter_context(nc.semaphore(f"s_{e}_{i}")) for i in range(max(n_ep, 1))]
        dsems = {}
        for e in self.ENG:
            if self.ndma[e]:
                dsems[e] = [self.es.enter_context(nc.semaphore(f"d_{e}_{i}")) for i in range(NDS)]

        def emit(e, eng):
            waited = {x: 0 for x in self.ENG}
            dwaited = {}
            for rec in self.ins[e]:
                need = {}
                for d in rec.deps:
                    if not self._needs_wait(rec, d):
                        continue
                    if d.dma:
                        key = (d.eng, d.dn % NDS)
                        val = 16 * (d.dn // NDS + 1)
                        if dwaited.get(key, 0) < val:
                            dwaited[key] = val
                            eng.wait_ge(dsems[d.eng][d.dn % NDS], val)
                    else:
                        if d.sigval > need.get(d.eng, 0):
                            need[d.eng] = d.sigval
                for x, sv in need.items():
                    if waited[x] < sv:
                        waited[x] = sv
                        eng.wait_ge(sems[x][(sv - 1) // EPOCH], (sv - 1) % EPOCH + 1)
                if rec.dma and rec.dn >= NDS:
                    key = (e, rec.dn % NDS)
                    val = 16 * (rec.dn // NDS)
                    if dwaited.get(key, 0) < val:
                        dwaited[key] = val
                        eng.wait_ge(dsems[e][rec.dn % NDS], val)
                ins = rec.fn(eng)
                if rec.dma:
                    ins.then_inc(dsems[e][rec.dn % NDS], 16)
                elif rec.signal:
                    ins.then_inc(sems[e][(rec.sigval - 1) // EPOCH], 1)
            n = self.ndma[e]
            if n:
                for k in range(NDS):
                    cnt = (n - k + NDS - 1) // NDS if n > k else 0
                    if cnt:
                        eng.wait_ge(dsems[e][k], 16 * cnt)

        with nc.Block() as block:
            if self.ins["pe"]:
                @block.tensor
                def _(eng):
                    emit("pe", eng)
            if self.ins["act"]:
                @block.scalar
                def _(eng):
                    emit("act", eng)
            if self.ins["dve"]:
                @block.vector
                def _(eng):
                    emit("dve", eng)
            if self.ins["pool"]:
                @block.gpsimd
                def _(eng):
                    emit("pool", eng)
            if self.ins["sp"]:
                @block.sync
                def _(eng):
                    emit("sp", eng)
        self.es.close()

import os

D = 2048
SEQ = 8192
CTX = 256
TB = 256
NB = TB + 4
NBLK = 1 + SEQ // TB
NCOL = NB + SEQ + 4
NTOK = CTX + SEQ
IN_A = 1024
IN_B = 4 * 768 + 24
A_W = 512
EPS = 1e-6
NFM = 30


def blk_info(n):
    if n == 0:
        return 0, 0, True, True, True
    m = n - 1
    return NB + TB * m, CTX + TB * m, False, m == 0, m == SEQ // TB - 1


def make_consts():
    c = {}
    c["ones"] = np.ones((128, 128), np.float32)
    c["ident"] = np.eye(128, dtype=np.float32)
    k = np.arange(64)
    tri = (k[:, None] <= k[None, :]).astype(np.float32)
    c["tri"] = tri
    low_incl = (k[:, None] >= k[None, :]).astype(np.float32)
    low_strict = (k[:, None] > k[None, :]).astype(np.float32)
    up_incl = (k[:, None] <= k[None, :]).astype(np.float32)
    c["m_low"] = np.tile(low_incl[:, None, :], (1, 6, 1))
    c["m_lows"] = np.tile(low_strict[:, None, :], (1, 6, 1))
    c["m_up"] = np.tile(up_incl[:, None, :], (1, 6, 1))
    c["id6"] = np.tile(np.eye(64, dtype=np.float32)[:, None, :], (1, 6, 1))
    return c


def rope_tables():
    row = np.repeat(np.arange(SEQ // 64, dtype=np.float32), 64)
    col = np.tile(np.arange(64, dtype=np.float32), SEQ // 64)
    inv = np.power(np.float32(10000.0), -np.arange(16, dtype=np.float32) / np.float32(16)).astype(np.float32)
    ar = row[:, None] * inv
    ac = col[:, None] * inv
    ang = np.concatenate([ar, ar, ac, ac], axis=-1)
    cos = np.cos(ang).astype(np.float32)
    sin = np.sin(ang).astype(np.float32)
    sgn = np.concatenate([-np.ones(16), np.ones(16), -np.ones(16), np.ones(16)]).astype(np.float32)
    return cos, sin * sgn


SWAP = np.concatenate([np.arange(16, 32), np.arange(0, 16), np.arange(48, 64), np.arange(32, 48)])


def mix_host(inp, li, r, xfull, ctxfull, xT_ready=None):
    b, dr = r // 2, r % 2
    f32 = np.float32
    if xT_ready is not None:
        xT = xT_ready
    else:
        xs = xfull[b]
        cs = ctxfull[b]
        if dr:
            xs = xs[::-1]
            cs = cs[::-1]
        xT = np.zeros((D, NCOL), f32)
        xT[:, 2:2 + CTX] = cs.T
        xT[:, NB + 2:NB + 2 + SEQ] = xs.T
    m = {"xT": xT}
    m["c2T"] = np.ascontiguousarray(np.stack([inp["c"][b], inp["c_ctx"]], axis=1))
    m["ada_w"] = np.ascontiguousarray(inp["ada_w"][li][:, :2 * D])
    m["ada_bT"] = np.ascontiguousarray(inp["ada_b"][li][:2 * D].reshape(32, 128).T)
    m["n1g"] = np.ascontiguousarray(inp["norm1_g"][li].reshape(16, 128).T)
    w_in = inp["w_in"][li]
    gs = [2 * dr, 2 * dr + 1]
    hs = [3 * dr + i for i in range(3)]
    cols = []
    for g in gs:
        cols.append(np.arange(g * 128, (g + 1) * 128))
    cols.append(IN_A + np.arange(2304))
    for h in hs:
        cols.append(IN_A + 3 * 768 + h * 128 + np.arange(128))
    cols.append(IN_A + IN_B + np.arange(768))
    cols.append(IN_A + IN_B + 768 + np.arange(64))
    cols.append(IN_A + IN_B + 768 + SWAP)
    cols = np.concatenate(cols)
    assert cols.shape[0] == NFM * 128
    m["w_fm"] = np.ascontiguousarray(w_in[:, cols])
    tcols = []
    for g in gs:
        tcols.append(A_W + g * 128 + np.arange(128))
    tcols.append(IN_A + 4 * 768 + dr * 6 + np.arange(6))
    tcols.append(IN_A + 4 * 768 + 12 + dr * 6 + np.arange(6))
    m["w_tm"] = np.ascontiguousarray(w_in[:, np.concatenate(tcols)])
    sg = inp["sgu_norm_g"][li].reshape(4, 128)[gs].reshape(1, 256)
    m["sgu_g"] = np.ascontiguousarray(np.broadcast_to(sg, (128, 256)))
    ws = inp["sgu_w"][li][gs]
    bs = inp["sgu_b"][li][gs]
    if dr:
        ws = ws[:, ::-1, ::-1]
        bs = bs[:, ::-1]
    m["wsT"] = np.ascontiguousarray(np.transpose(ws, (2, 0, 1)))
    m["bs"] = np.ascontiguousarray(bs.reshape(1, 2, 128))
    cw = inp["dn_conv_w"][li]
    if dr:
        cw = cw[::-1]
    m["convw"] = np.ascontiguousarray(cw.reshape(5, 18, 128).transpose(2, 1, 0))
    m["alog"] = np.ascontiguousarray(np.broadcast_to(inp["dn_a_log"][li][dr].reshape(1, 1, 6), (64, 4, 6)))
    m["dtb"] = np.ascontiguousarray(np.broadcast_to(inp["dn_dt_bias"][li][dr].reshape(1, 1, 6), (64, 4, 6)))
    m["qng"] = np.ascontiguousarray(inp["mla_q_norm_g"][li].reshape(4, 128).T)
    m["kvng"] = np.ascontiguousarray(inp["mla_kv_norm_g"][li].reshape(2, 128).T)
    wuq = inp["mla_w_uq"][li]
    qc = []
    for h in hs:
        qc.append(h * 192 + np.arange(128))
        qc.append(h * 192 + 128 + np.arange(64))
        qc.append(h * 192 + 128 + SWAP)
    m["wuq"] = np.ascontiguousarray(wuq[:, np.concatenate(qc)])
    wukv = inp["mla_w_ukv"][li]
    kc = [h * 256 + np.arange(128) for h in hs] + [h * 256 + 128 + np.arange(128) for h in hs]
    m["wukv"] = np.ascontiguousarray(wukv[:, np.concatenate(kc)])
    cos, sins = rope_tables()
    if dr:
        cos = cos[::-1]
        sins = sins[::-1]
    cT = np.ones((64, NTOK), f32)
    sT = np.zeros((64, NTOK), f32)
    cT[:, CTX:] = cos.T
    sT[:, CTX:] = sins.T
    m["cosT"] = cT
    m["sinT"] = sT
    for k, v in make_consts().items():
        m["c_" + k] = v
    return m


QB = 512
NKT = NTOK // 128
ASCALE = 192.0 ** -0.5


def build_mix(stages=("sgu", "dn", "mla", "attn"), nblk=NBLK):
    nc = bass.Bass("TRN2", target_bir_lowering=False)
    p = Prog(nc)

    def din(name, shape):
        return nc.dram_tensor(name, list(shape), F32, kind="ExternalInput").ap()

    def dout(name, shape):
        return nc.dram_tensor(name, list(shape), F32, kind="ExternalOutput").ap()

    xT = din("xT", [D, NCOL])
    c2T = din("c2T", [D, 2])
    ada_w = din("ada_w", [D, 2 * D])
    ada_bT = din("ada_bT", [128, 32])
    n1g = din("n1g", [128, 16])
    w_fm = din("w_fm", [D, NFM * 128])
    w_tm = din("w_tm", [D, 268])
    sgu_g = din("sgu_g", [128, 256])
    wsT_d = din("wsT", [128, 2, 128])
    bs_d = din("bs", [1, 2, 128])
    convw_d = din("convw", [128, 18, 5])
    alog_d = din("alog", [64, 4, 6])
    dtb_d = din("dtb", [64, 4, 6])
    qng_d = din("qng", [128, 4])
    kvng_d = din("kvng", [128, 2])
    wuq_d = din("wuq", [512, 768])
    wukv_d = din("wukv", [256, 768])
    cosT_d = din("cosT", [64, NTOK])
    sinT_d = din("sinT", [64, NTOK])
    c_ones = din("c_ones", [128, 128])
    c_ident = din("c_ident", [128, 128])
    c_tri = din("c_tri", [64, 64])
    c_mlow = din("c_m_low", [64, 6, 64])
    c_mlows = din("c_m_lows", [64, 6, 64])
    c_mup = din("c_m_up", [64, 6, 64])
    c_id6 = din("c_id6", [64, 6, 64])

    outA = dout("outA", [256, NTOK])
    oT = dout("oT", [768, NTOK])
    sgate = dout("sgate", [384, NTOK])
    outC = dout("outC", [384, NTOK])
    dbg = dout("dbg", [128, 64, 260]) if any(st_.startswith("dbg") for st_ in stages) else None

    Ksn = nc.dram_tensor("Ksn", [128, 3, NTOK], BF16, kind="Internal").ap()
    Ksp = nc.dram_tensor("Ksp", [64, NTOK], BF16, kind="Internal").ap()
    Vsc = nc.dram_tensor("Vsc", [128, NKT, 384], BF16, kind="Internal").ap()
    Qsn = nc.dram_tensor("Qsn", [128, 3, NTOK], BF16, kind="Internal").ap()
    Qsp = nc.dram_tensor("Qsp", [64, 3, NTOK], BF16, kind="Internal").ap()

    ones_bf = p.sb([128, 128], BF16)
    p.dma(ones_bf[:], c_ones, q="pool")
    ident = p.sb([128, 128])
    p.dma(ident[:], c_ident)
    epsb = p.sb([128, 1])
    p.op("dve", lambda e: e.memset(epsb[:].ap, EPS), [], [epsb])
    PS = [p.ps([128, 512]) for _ in range(8)]

    es1 = ExitStack()

    def sb(shape, dtype=F32):
        return p.sb(shape, dtype, es=es1)

    ones_f = sb([128, 128])
    p.dma(ones_f[:], c_ones)
    tri = sb([64, 64])
    p.dma(tri[:], c_tri)
    m_lows = sb([64, 3, 64])
    p.dma(m_lows[:], c_mlows[:, 0:3, :])
    m_up = sb([64, 3, 64])
    p.dma(m_up[:], c_mup[:, 0:3, :])
    id3 = sb([64, 3, 64])
    p.dma(id3[:], c_id6[:, 0:3, :])
    cw = sb([128, 18, 5])
    p.dma(cw[:], convw_d)
    alog = sb([64, 4, 6])
    p.dma(alog[:], alog_d)
    dtb = sb([64, 4, 6])
    p.dma(dtb[:], dtb_d)
    nea = sb([64, 4, 6])
    p.I("act", "activation", out=nea[:], in_=alog[:], func=AF.Exp)
    p.I("dve", "tensor_scalar", out=nea[:], in0=nea[:], scalar1=-1.0, scalar2=None, op0=ALU.mult)
    qng = sb([128, 4])
    p.dma(qng[:], qng_d)
    kvng = sb([128, 2])
    p.dma(kvng[:], kvng_d)
    wuq = sb([128, 4, 768], BF16)
    p.dma(wuq[:], wuq_d.rearrange("(c p) n -> p c n", p=128), q="pool")
    wukv = sb([128, 2, 768], BF16)
    p.dma(wukv[:], wukv_d.rearrange("(c p) n -> p c n", p=128), q="pool")

    c2 = sb([128, 16, 2])
    p.dma(c2[:], c2T.rearrange("(c p) n -> p c n", p=128))
    sc2 = sb([128, 16, 2])
    p.I("act", "activation", out=sc2[:], in_=c2[:], func=AF.Silu)
    adab = sb([128, 32])
    p.dma(adab[:], ada_bT)
    n1 = sb([128, 16])
    p.dma(n1[:], n1g)
    modT = sb([128, 32, 2])
    qkv = sb([128, 18, TB])
    awt = [qkv[:, 0:8, :].rearrange("p a (b c) -> p (a b) c", c=128), qkv[:, 8:16, :].rearrange("p a (b c) -> p (a b) c", c=128)]
    BC = PS[0]
    for j in range(32):
        w = awt[j % 2]
        p.dma(w, ada_w[:, j * 128:(j + 1) * 128].rearrange("(c p) n -> p c n", p=128))
        for k in range(16):
            p.mm(BC[:, 2 * (j % 2):2 * (j % 2) + 2], w[:, k, :], sc2[:, k, :], k == 0, k == 15)
        p.I("dve", "tensor_scalar", out=modT[:, j, :], in0=BC[:, 2 * (j % 2):2 * (j % 2) + 2], scalar1=adab[:, j:j + 1],
            scalar2=None, op0=ALU.add)
    gsT = sb([128, 16, 2])
    for s in range(2):
        p.I("dve", "scalar_tensor_tensor", out=gsT[:, :, s], in0=modT[:, 16:32, s], scalar=1.0, in1=n1[:], op0=ALU.add, op1=ALU.mult)

    wtm = sb([128, 16, 268], BF16)
    p.dma(wtm[:], w_tm.rearrange("(c p) n -> p c n", p=128), q="pool")
    sgg = sb([128, 256])
    p.dma(sgg[:], sgu_g)
    wsT = sb([128, 2, 128], BF16)
    p.dma(wsT[:], wsT_d, q="pool")
    bsb = sb([1, 2, 128], BF16)
    p.dma(bsb[:], bs_d, q="pool")

    xt = [sb([128, NB]) for _ in range(4)]
    hT = sb([128, 16, NB], BF16)
    sqb = [sb([128, NB], BF16) for _ in range(2)]
    tmpf = [sb([128, NB]) for _ in range(2)]
    rstd = sb([128, NB])
    wt = [sb([128, 16, 128], BF16) for _ in range(3)]
    Z = [PS[1], PS[2]]
    TMp = PS[3]
    W = PS[4:8]
    wrot = [0]

    def nxt():
        w = W[wrot[0] % 4]
        wrot[0] += 1
        return w
    uT = sb([128, 2, TB])
    zq = sb([128, 18, NB], BF16)
    sg = sb([128, 3, TB])
    ql = sb([128, 4, TB])
    kvl = sb([128, 2, TB])
    kp = sb([64, 2, TB])
    gv = sb([128, 256])
    cen = sb([128, 256])
    junk = sb([128, 128])
    st1 = sb([128, 8])
    vn = sb([128, 256], BF16)
    oa = sb([128, 2, TB])
    sqq = sb([128, 2, TB], BF16)
    rinv = sb([128, 2, TB])
    qkb = sb([128, 12, TB], BF16)
    ab4 = sb([64, 4, 12])
    g4 = sb([64, 4, 6])
    beta4 = sb([64, 4, 6])
    osb = sb([128, 6, TB])
    Sf = [sb([128, 3, 128]) for _ in range(2)]
    Sb = [sb([128, 3, 128], BF16) for _ in range(2)]
    for hg in range(2):
        p.op("pool", lambda e, t=Sf[hg]: e.memset(t[:].ap, 0.0), [], [Sf[hg]])
        p.op("pool", lambda e, t=Sb[hg]: e.memset(t[:].ap, 0.0), [], [Sb[hg]])

    def dset():
        d = {}
        d["ktm"] = sb([64, 3, 128]); d["vtm"] = sb([64, 3, 128])
        d["Pm"] = sb([64, 3, 64]); d["gcol"] = sb([64, 3]); d["EGB"] = sb([128, 3, 64])
        d["D1s"] = sb([64, 3, 64]); d["D2"] = sb([64, 3, 64])
        d["egc"] = sb([64, 3]); d["be"] = sb([64, 3]); d["nbeta"] = sb([64, 3]); d["dl"] = sb([64, 3]); d["ekl"] = sb([64, 3])
        d["Msb"] = sb([64, 3, 64]); d["qkT"] = sb([64, 3, 64], BF16)
        d["PP"] = [sb([64, 2, 3, 64], BF16) for _ in range(2)]
        d["TT"] = [sb([64, 3, 64], BF16) for _ in range(2)]
        d["vb"] = sb([64, 3, 128], BF16); d["kbg"] = sb([64, 3, 128], BF16); d["kg"] = sb([64, 3, 128], BF16)
        d["u"] = sb([64, 3, 128]); d["wT"] = sb([128, 3, 64], BF16); d["qgT"] = sb([128, 3, 64], BF16)
        d["vnew"] = sb([64, 3, 128], BF16)
        return d
    dnl = 0
    for st_ in stages:
        if st_ == "dn":
            dnl = 9
        elif st_.startswith("dn:"):
            dnl = int(st_[3:])
    if dnl:
        stages = tuple(stages) + ("dn",)
    DS = [dset() for hg in range(2)] if "dn" in stages else None
    if "mla" in stages:
        sq4 = sb([128, 4, TB], BF16)
        rq = sb([128, TB])
        cqT = sb([128, 4, TB], BF16)
        ckvT = sb([128, 2, TB], BF16)
        cosb = sb([64, TB]); sinb = sb([64, TB])
        qn_sb = sb([128, 3, TB], BF16); qp_sb = sb([64, 3, TB], BF16)
        kn_sb = sb([128, 3, TB], BF16); kp_sb = sb([64, TB], BF16)
        v_sb = sb([128, 2, 384], BF16)
        t1 = sb([64, TB]); t2 = sb([64, TB])
    p.barrier()
    wcnt = 0

    for n in range(nblk):
        c0, t0, is_ctx, first, last = blk_info(n)
        s = 1 if is_ctx else 0
        h = hT
        for c in range(16):
            x = xt[c % 4]
            p.dma(x[:], xT[c * 128:(c + 1) * 128, c0:c0 + NB])
            sq = sqb[c % 2]
            p.I("act", "activation", out=sq[:], in_=x[:], func=AF.Square)
            p.mm(BC[:, 0:NB], ones_bf[:], sq[:], c == 0, c == 15)
        p.I("act", "activation", out=rstd[:], in_=BC[:, 0:NB], func=AF.Sqrt, scale=1.0 / D, bias=epsb[:])
        p.I("dve", "reciprocal", out=rstd[:], in_=rstd[:])
        for c in range(16):
            x = xt[c % 4]
            p.dma(x[:], xT[c * 128:(c + 1) * 128, c0:c0 + NB])
            tf = tmpf[c % 2]
            p.I("dve", "tensor_tensor", out=tf[:], in0=x[:], in1=rstd[:], op=ALU.mult)
            p.I("act", "activation", out=h[:, c, :], in_=tf[:], func=AF.Identity, scale=gsT[:, c, s:s + 1], bias=modT[:, c, s:s + 1])
        if first:
            p.op("pool", lambda e, h=h: e.memset(h[:, :, 0:2].ap, 0.0), [], [h])
        if last:
            p.op("pool", lambda e, h=h: e.memset(h[:, :, NB - 2:NB].ap, 0.0), [], [h])
        for j in range(NFM):
            w = wt[wcnt % 3]
            wcnt += 1
            p.dma(w[:], w_fm[:, j * 128:(j + 1) * 128].rearrange("(c p) n -> p c n", p=128), q="pool")
            zp = Z[j % 2]
            if j < NFM - 1:
                for k in range(16):
                    p.mm(zp[:, 0:NB], w[:, k, :], h[:, k, :], k == 0, k == 15)
            else:
                for half in range(2):
                    for k in range(16):
                        p.mm(zp[0:64, half * TB:(half + 1) * TB], w[:, k, half * 64:(half + 1) * 64], h[:, k, 2:2 + TB], k == 0, k == 15)
            if j < 2:
                p.I("act", "activation", out=uT[:, j, :], in_=zp[:, 2:2 + TB], func=AF.Gelu_apprx_tanh)
            elif j < 20:
                p.I("act", "activation", out=zq[:, j - 2, :], in_=zp[:, 0:NB], func=AF.Identity)
            elif j < 23:
                p.I("act", "activation", out=sg[:, j - 20, :], in_=zp[:, 2:2 + TB], func=AF.Silu)
            elif j < 27:
                p.I("dve", "tensor_copy", out=ql[:, j - 23, :], in_=zp[:, 2:2 + TB])
            elif j < 29:
                p.I("dve", "tensor_copy", out=kvl[:, j - 27, :], in_=zp[:, 2:2 + TB])
            else:
                p.I("dve", "tensor_copy", out=kp[:, :, :], in_=zp[0:64, 0:2 * TB].rearrange("p (a b) -> p a b", a=2))
        p.dma(sgate[:, t0:t0 + TB].rearrange("(c p) n -> p c n", p=128), sg[:])
        if "sgu" in stages:
            for sb_ in range(2):
                tok = slice(2 + 128 * sb_, 2 + 128 * (sb_ + 1))
                for k in range(16):
                    p.mm(TMp[:, 0:256], h[:, k, tok], wtm[:, k, 0:256], k == 0, k == 15)
                p.I("act", "activation", out=gv[:], in_=TMp[:, 0:256], func=AF.Gelu_apprx_tanh)
                p.I("dve", "tensor_reduce", out=st1[:, 0:2], in_=gv[:].rearrange("p (g c) -> p g c", g=2), axis=AX.X, op=ALU.add)
                p.I("dve", "tensor_scalar", out=st1[:, 2:4], in0=st1[:, 0:2], scalar1=-1.0 / 128, scalar2=None, op0=ALU.mult)
                for g in range(2):
                    gsl = slice(g * 128, (g + 1) * 128)
                    p.I("act", "activation", out=junk[:], in_=gv[:, gsl], func=AF.Square, bias=st1[:, 2 + g:3 + g], accum_out=st1[:, 4 + g:5 + g])
                    p.I("dve", "tensor_scalar", out=cen[:, gsl], in0=gv[:, gsl], scalar1=st1[:, 2 + g:3 + g], scalar2=None, op0=ALU.add)
                p.I("act", "activation", out=st1[:, 6:8], in_=st1[:, 4:6], func=AF.Sqrt, scale=1.0 / 128, bias=epsb[:])
                p.I("dve", "reciprocal", out=st1[:, 6:8], in_=st1[:, 6:8])
                for g in range(2):
                    gsl = slice(g * 128, (g + 1) * 128)
                    p.I("dve", "scalar_tensor_tensor", out=vn[:, gsl], in0=cen[:, gsl], scalar=st1[:, 6 + g:7 + g], in1=sgg[:, gsl], op0=ALU.mult, op1=ALU.mult)
                    msl = slice(256 + g * 128, 256 + (g + 1) * 128)
                    p.mm(TMp[:, msl], vn[:, gsl], wsT[:, g, :], True, False)
                    p.mm(TMp[:, msl], ones_bf[0:1, :], bsb[0:1, g, :], False, True)
                    p.I("dve", "tensor_tensor", out=oa[:, g, 128 * sb_:128 * (sb_ + 1)], in0=uT[:, g, 128 * sb_:128 * (sb_ + 1)], in1=TMp[:, msl], op=ALU.mult)
            p.dma(outA[:, t0:t0 + TB].rearrange("(c p) n -> p c n", p=128), oa[:])

        if "dn" in stages:
            for i in range(5):
                for j in range(0 if "noconv" in os.environ.get("MIXDBG", "") else 18):
                    if i == 0:
                        p.I("dve", "tensor_scalar", out=qkv[:, j, :], in0=zq[:, j, 0:TB], scalar1=cw[:, j, 0:1], scalar2=None, op0=ALU.mult)
                    else:
                        p.I("dve", "scalar_tensor_tensor", out=qkv[:, j, :], in0=zq[:, j, i:i + TB], scalar=cw[:, j, i:i + 1], in1=qkv[:, j, :], op0=ALU.mult, op1=ALU.add)
            pass
            DBG = os.environ.get("MIXDBG", "")
            for a3 in range(0 if "nosilu" in DBG else 3):
                p.I("act", "activation", out=qkv[:, 6 * a3:6 * a3 + 6, :], in_=qkv[:, 6 * a3:6 * a3 + 6, :], func=AF.Silu)
            for j2 in range(0 if "nonorm" in DBG else 6):
                p.I("act", "activation", out=sqq[:], in_=qkv[:, 2 * j2:2 * j2 + 2, :], func=AF.Square)
                wp = nxt()
                for jj in range(2):
                    p.mm(wp[:, jj * TB:(jj + 1) * TB], ones_bf[:], sqq[:, jj, :], True, True)
                p.I("act", "activation", out=rinv[:], in_=wp[:, :].rearrange("p (a b) -> p a b", a=2), func=AF.Sqrt, scale=1.0, bias=epsb[:])
                p.I("dve", "reciprocal", out=rinv[:], in_=rinv[:])
                if j2 < 3:
                    p.I("dve", "scalar_tensor_tensor", out=qkv[:, 2 * j2:2 * j2 + 2, :], in0=qkv[:, 2 * j2:2 * j2 + 2, :], scalar=128.0 ** -0.5, in1=rinv[:], op0=ALU.mult, op1=ALU.mult)
                else:
                    p.I("dve", "tensor_tensor", out=qkv[:, 2 * j2:2 * j2 + 2, :], in0=qkv[:, 2 * j2:2 * j2 + 2, :], in1=rinv[:], op=ALU.mult)
            p.I("pool", "tensor_copy", out=qkb[:], in_=qkv[:, 0:12, :])
            for cc in range(4 if dnl >= 2 else 0):
                tok = slice(2 + 64 * cc, 2 + 64 * (cc + 1))
                for k in range(16):
                    p.mm(BC[0:64, 264 + cc * 12:264 + (cc + 1) * 12], h[:, k, tok], wtm[:, k, 256:268], k == 0, k == 15)
            if dnl >= 2:
                p.I("dve", "tensor_copy", out=ab4[:], in_=BC[0:64, 264:312].rearrange("p (a b) -> p a b", a=4))
                p.I("dve", "tensor_tensor", out=g4[:], in0=ab4[:, :, 0:6], in1=dtb[:], op=ALU.add)
                p.I("act", "activation", out=g4[:], in_=g4[:], func=AF.Exp)
                p.I("act", "activation", out=g4[:], in_=g4[:], func=AF.Ln, bias=1.0, scale=1.0)
                p.I("dve", "tensor_tensor", out=g4[:], in0=g4[:], in1=nea[:], op=ALU.mult)
                p.I("act", "activation", out=beta4[:], in_=ab4[:, :, 6:12], func=AF.Sigmoid)
            for cc in range(4 if dnl >= 3 else 0):
                tk = slice(cc * 64, (cc + 1) * 64)
                def dn_iter(cc, hg, tk):
                    d = DS[hg]
                    hs3 = [3 * hg + i for i in range(3)]
                    g3 = g4[:, cc, 3 * hg:3 * hg + 3]
                    b3 = beta4[:, cc, 3 * hg:3 * hg + 3]
                    yield
                    wa = nxt()
                    for hh, hd in enumerate(hs3):
                        p.tr(wa[0:64, hh * 128:(hh + 1) * 128], qkv[:, 6 + hd, tk], ident[:])
                    p.I("act", "activation", out=d["ktm"][:], in_=wa[0:64, 0:384].rearrange("p (a b) -> p a b", a=3), func=AF.Identity)
                    yield
                    wb = nxt()
                    for hh, hd in enumerate(hs3):
                        p.tr(wb[0:64, hh * 128:(hh + 1) * 128], qkv[:, 12 + hd, tk], ident[:])
                    p.I("act", "activation", out=d["vtm"][:], in_=wb[0:64, 0:384].rearrange("p (a b) -> p a b", a=3), func=AF.Identity)
                    if dnl < 4:
                        return
                    L4 = int(os.environ.get("L4", "9"))
                    for hh in range(3):
                        p.I("dve", "tensor_scalar", out=d["Pm"][:, hh, :], in0=tri[:], scalar1=g3[:, hh:hh + 1], scalar2=None, op0=ALU.mult)
                    yield
                    wc = nxt()
                    p.mm(wc[:, 0:192], ones_f[0:64, :], d["Pm"][:].rearrange("p a b -> p (a b)"), True, True)
                    if L4 >= 2:
                        p.mm(wc[0:64, 192:195], tri[:], g3, True, True)
                        p.I("dve", "tensor_copy", out=d["gcol"][:], in_=wc[0:64, 192:195])
                    if L4 >= 3:
                        p.I("act", "activation", out=d["EGB"][:], in_=wc[:, 0:192].rearrange("p (a b) -> p a b", a=3), func=(AF.Identity if os.environ.get("EGBID") else AF.Exp))
                    if L4 >= 4:
                        for hh in range(3):
                            p.I("dve", "tensor_scalar", out=d["D1s"][:, hh, :], in0=wc[0:64, hh * 64:(hh + 1) * 64], scalar1=d["gcol"][:, hh:hh + 1], scalar2=0.0, op0=ALU.subtract, op1=ALU.max)
                            p.I("dve", "tensor_scalar", out=d["D2"][:, hh, :], in0=wc[0:64, hh * 64:(hh + 1) * 64], scalar1=d["gcol"][:, hh:hh + 1], scalar2=0.0, op0=ALU.subtract, op1=ALU.min)
                    if L4 >= 5:
                        p.I("dve", "tensor_tensor", out=d["dl"][:], in0=wc[0:64, 0:192].rearrange("p (a b) -> p a b", a=3)[:, :, 63], in1=d["gcol"][:], op=ALU.subtract)
                    if L4 >= 6:
                        p.I("act", "activation", out=d["D1s"][:], in_=d["D1s"][:], func=AF.Exp, scale=-1.0)
                        p.I("act", "activation", out=d["D2"][:], in_=d["D2"][:], func=AF.Exp)
                        p.I("act", "activation", out=d["egc"][:], in_=d["gcol"][:], func=AF.Exp)
                        p.I("act", "activation", out=d["ekl"][:], in_=d["dl"][:], func=AF.Exp)
                    if L4 >= 7:
                        p.I("pool", "tensor_tensor", out=d["D1s"][:], in0=d["D1s"][:], in1=m_lows[:], op=ALU.mult)
                        p.I("pool", "tensor_tensor", out=d["D2"][:], in0=d["D2"][:], in1=m_up[:], op=ALU.mult)
                    if L4 >= 8:
                        p.I("dve", "tensor_tensor", out=d["be"][:], in0=b3, in1=d["egc"][:], op=ALU.mult)
                        p.I("dve", "tensor_scalar", out=d["nbeta"][:], in0=b3, scalar1=-1.0, scalar2=None, op0=ALU.mult)
                    if dnl < 5:
                        return
                    yield
                    wd = nxt()
                    for hh, hd in enumerate(hs3):
                        p.mm(wd[0:64, hh * 64:(hh + 1) * 64], qkb[:, 6 + hd, tk], qkb[:, 6 + hd, tk], True, True)
                    for hh, hd in enumerate(hs3):
                        p.mm(wd[0:64, 192 + hh * 64:192 + (hh + 1) * 64], qkb[:, 6 + hd, tk], qkb[:, hd, tk], True, True)
                    for hh in range(3):
                        p.I("dve", "scalar_tensor_tensor", out=d["Msb"][:, hh, :], in0=wd[0:64, hh * 64:(hh + 1) * 64], scalar=d["nbeta"][:, hh:hh + 1], in1=d["D1s"][:, hh, :], op0=ALU.mult, op1=ALU.mult)
                    p.I("dve", "tensor_tensor", out=d["qkT"][:], in0=wd[0:64, 192:384].rearrange("p (a b) -> p a b", a=3), in1=d["D2"][:], op=ALU.mult)
                    if dnl < 6:
                        return
                    yield
                    PP = d["PP"]; TT = d["TT"]
                    yield
                    we = nxt()
                    for hh in range(3):
                        p.tr(we[0:64, hh * 64:(hh + 1) * 64], d["Msb"][:, hh, :], ident[0:64, 0:64])
                    p.I("act", "activation", out=PP[0][:, 1, :, :], in_=we[0:64, 0:192].rearrange("p (a b) -> p a b", a=3), func=AF.Identity)
                    p.I("pool", "tensor_copy", out=PP[0][:, 0, :, :], in_=d["Msb"][:])
                    p.I("pool", "tensor_tensor", out=TT[0][:], in0=PP[0][:, 1, :, :], in1=id3[:], op=ALU.add)
                    for lev in range(1, 6):
                        Po = PP[(lev - 1) % 2]; Pn = PP[lev % 2]
                        To = TT[(lev - 1) % 2]; Tn = TT[lev % 2]
                        yield
                        wx = nxt()
                        for hh in range(3):
                            p.mm(wx[0:64, hh * 64:(hh + 1) * 64], Po[:, 1, hh, :], Po[:, 0, hh, :], True, True)
                        if lev < 5:
                            for hh in range(3):
                                p.mm(wx[0:64, 192 + hh * 64:192 + (hh + 1) * 64], Po[:, 0, hh, :], Po[:, 1, hh, :], True, True)
                            p.I("act", "activation", out=Pn[:].rearrange("p a b c -> p (a b c)"), in_=wx[0:64, 0:384], func=AF.Identity)
                        else:
                            p.I("act", "activation", out=Pn[:, 0, :, :].rearrange("p b c -> p (b c)"), in_=wx[0:64, 0:192], func=AF.Identity)
                        yield
                        wy = nxt()
                        for hh in range(3):
                            p.mm(wy[0:64, hh * 64:(hh + 1) * 64], Pn[:, 0, hh, :], To[:, hh, :], True, True)
                        p.I("dve", "tensor_tensor", out=Tn[:].rearrange("p a b -> p (a b)"), in0=To[:].rearrange("p a b -> p (a b)"), in1=wy[0:64, 0:192], op=ALU.add)
                    TTf = TT[5 % 2]
                    if dnl < 7:
                        return
                    yield
                    for hh in range(3):
                        p.I("act", "activation", out=d["vb"][:, hh, :], in_=d["vtm"][:, hh, :], func=AF.Identity, scale=b3[:, hh:hh + 1])
                        p.I("act", "activation", out=d["kbg"][:, hh, :], in_=d["ktm"][:, hh, :], func=AF.Identity, scale=d["be"][:, hh:hh + 1])
                        p.I("dve", "tensor_scalar", out=d["kg"][:, hh, :], in0=d["ktm"][:, hh, :], scalar1=d["ekl"][:, hh:hh + 1], scalar2=None, op0=ALU.mult)
                    yield
                    wu = nxt()
                    for hh in range(3):
                        p.mm(wu[0:64, hh * 128:(hh + 1) * 128], TTf[:, hh, :], d["vb"][:, hh, :], True, True)
                    p.I("act", "activation", out=d["u"][:].rearrange("p a b -> p (a b)"), in_=wu[0:64, 0:384], func=AF.Identity)
                    yield
                    ww = nxt()
                    for hh in range(3):
                        p.mm(ww[:, hh * 64:(hh + 1) * 64], d["kbg"][:, hh, :], TTf[:, hh, :], True, True)
                    p.I("dve", "tensor_copy", out=d["wT"][:].rearrange("p a b -> p (a b)"), in_=ww[:, 0:192])
                    p.I("dve", "tensor_tensor", out=d["qgT"][:], in0=qkv[:, 3 * hg:3 * hg + 3, tk], in1=d["EGB"][:], op=ALU.mult)
                    if dnl < 8:
                        return
                    yield
                    S_f = Sf[hg]; S_b = Sb[hg]
                    yield
                    ws_ = nxt()
                    for hh in range(3):
                        p.mm(ws_[0:64, hh * 128:(hh + 1) * 128], d["wT"][:, hh, :], S_b[:, hh, :], True, True)
                    p.I("dve", "tensor_tensor", out=d["vnew"][:].rearrange("p a b -> p (a b)"), in0=d["u"][:].rearrange("p a b -> p (a b)"), in1=ws_[0:64, 0:384], op=ALU.subtract)
                    yield
                    wo = nxt()
                    for hh in range(3):
                        p.mm(wo[:, hh * 64:(hh + 1) * 64], S_b[:, hh, :], d["qgT"][:, hh, :], True, False)
                        p.mm(wo[:, hh * 64:(hh + 1) * 64], d["vnew"][:, hh, :], d["qkT"][:, hh, :], False, True)
                    yield
                    wt_ = nxt()
                    for hh in range(3):
                        p.mm(wt_[:, hh * 128:(hh + 1) * 128], d["kg"][:, hh, :], d["vnew"][:, hh, :], True, True)
                    p.I("act", "activation", out=osb[:, 3 * hg:3 * hg + 3, tk], in_=wo[:, 0:192].rearrange("p (a b) -> p a b", a=3), func=AF.Identity)
                    for hh in range(3):
                        p.I("dve", "scalar_tensor_tensor", out=S_f[:, hh, :], in0=S_f[:, hh, :], scalar=d["EGB"][:, hh, 63:64], in1=wt_[:, hh * 128:(hh + 1) * 128], op0=ALU.mult, op1=ALU.add)
                    p.I("act", "activation", out=S_b[:], in_=S_f[:], func=AF.Identity)

                gens = [dn_iter(cc, 0, tk), dn_iter(cc, 1, tk)]
                while gens:
                    for g_ in list(gens):
                        try:
                            next(g_)
                        except StopIteration:
                            gens.remove(g_)
            for hg in range(2):
                p.dma(oT[384 * hg:384 * (hg + 1), t0:t0 + TB].rearrange("(c p) n -> p c n", p=128), osb[:, 3 * hg:3 * hg + 3, :])
            if "dbgq" in stages and n == 1:
                for j in range(18):
                    p.dma(dbg[:, j, 0:TB], qkv[:, j, :])
                if dnl >= 2:
                    p.dma(dbg[0:64, 18, 0:24], g4[:].rearrange("p a b -> p (a b)"))
                    p.dma(dbg[0:64, 19, 0:24], beta4[:].rearrange("p a b -> p (a b)"))

        if "mla" in stages:
            p.dma(cosb[:], cosT_d[:, t0:t0 + TB])
            p.dma(sinb[:], sinT_d[:, t0:t0 + TB])
            p.I("act", "activation", out=sq4[:], in_=ql[:], func=AF.Square)
            wp = nxt()
            for c in range(4):
                p.mm(wp[:, 0:TB], ones_bf[:], sq4[:, c, :], c == 0, c == 3)
            p.I("act", "activation", out=rq[:], in_=wp[:, 0:TB], func=AF.Sqrt, scale=1.0 / 512, bias=epsb[:])
            p.I("dve", "reciprocal", out=rq[:], in_=rq[:])
            for c in range(4):
                p.I("dve", "scalar_tensor_tensor", out=cqT[:, c, :], in0=ql[:, c, :], scalar=qng[:, c:c + 1], in1=rq[:], op0=ALU.mult, op1=ALU.mult)
            p.I("act", "activation", out=sq4[:, 0:2, :], in_=kvl[:], func=AF.Square)
            wp = nxt()
            for c in range(2):
                p.mm(wp[:, 0:TB], ones_bf[:], sq4[:, c, :], c == 0, c == 1)
            p.I("act", "activation", out=rq[:], in_=wp[:, 0:TB], func=AF.Sqrt, scale=1.0 / 256, bias=epsb[:])
            p.I("dve", "reciprocal", out=rq[:], in_=rq[:])
            for c in range(2):
                p.I("dve", "scalar_tensor_tensor", out=ckvT[:, c, :], in0=kvl[:, c, :], scalar=kvng[:, c:c + 1], in1=rq[:], op0=ALU.mult, op1=ALU.mult)
            for hh in range(3):
                wq = nxt()
                for k in range(4):
                    p.mm(wq[:, 0:TB], wuq[:, k, hh * 256:hh * 256 + 128], cqT[:, k, :], k == 0, k == 3)
                p.I("act", "activation", out=qn_sb[:, hh, :], in_=wq[:, 0:TB], func=AF.Identity)
                wq2 = nxt()
                for half in range(2):
                    for k in range(4):
                        p.mm(wq2[0:64, half * TB:(half + 1) * TB], wuq[:, k, hh * 256 + 128 + half * 64:hh * 256 + 192 + half * 64], cqT[:, k, :], k == 0, k == 3)
                p.I("dve", "tensor_tensor", out=t1[:], in0=wq2[0:64, 0:TB], in1=cosb[:], op=ALU.mult)
                p.I("dve", "tensor_tensor", out=t2[:], in0=wq2[0:64, TB:2 * TB], in1=sinb[:], op=ALU.mult)
                p.I("dve", "tensor_tensor", out=qp_sb[:, hh, :], in0=t1[:], in1=t2[:], op=ALU.add)
            for hh in range(3):
                wk = nxt()
                for k in range(2):
                    p.mm(wk[:, 0:TB], wukv[:, k, hh * 128:(hh + 1) * 128], ckvT[:, k, :], k == 0, k == 1)
                p.I("act", "activation", out=kn_sb[:, hh, :], in_=wk[:, 0:TB], func=AF.Identity)
            p.I("dve", "tensor_tensor", out=t1[:], in0=kp[:, 0, :], in1=cosb[:], op=ALU.mult)
            p.I("dve", "tensor_tensor", out=t2[:], in0=kp[:, 1, :], in1=sinb[:], op=ALU.mult)
            p.I("dve", "tensor_tensor", out=kp_sb[:], in0=t1[:], in1=t2[:], op=ALU.add)
            for sb_ in range(2):
                wv = nxt()
                for k in range(2):
                    p.mm(wv[:, 0:384], ckvT[:, k, 128 * sb_:128 * (sb_ + 1)], wukv[:, k, 384:768], k == 0, k == 1)
                p.I("act", "activation", out=v_sb[:, sb_, :], in_=wv[:, 0:384], func=AF.Identity)
            p.dma(Ksn[:, :, t0:t0 + TB], kn_sb[:])
            p.dma(Ksp[:, t0:t0 + TB], kp_sb[:])
            p.dma(Vsc[:, t0 // 128:t0 // 128 + 2, :], v_sb[:])
            p.dma(Qsn[:, :, t0:t0 + TB], qn_sb[:])
            p.dma(Qsp[:, :, t0:t0 + TB], qp_sb[:])

    es1.close()
    p.barrier()

    if "attn" in stages:
        es2 = ExitStack()

        def sb2(shape, dtype=F32):
            return p.sb(shape, dtype, es=es2)
        nkt = NKT if nblk == NBLK else (nblk * TB) // 128
        ntk = nkt * 128
        Kn = sb2([128, 3, NTOK], BF16)
        Kp = sb2([64, NTOK], BF16)
        Vt = sb2([128, NKT, 384], BF16)
        for hh in range(3):
            p.dma(Kn[:, hh, 0:ntk], Ksn[:, hh, 0:ntk])
        p.dma(Kp[:, 0:ntk], Ksp[:, 0:ntk])
        for a in range(0, nkt, 16):
            b = min(nkt, a + 16)
            p.dma(Vt[:, a:b, :], Vsc[:, a:b, :])
        Qn = [sb2([128, 3, QB], BF16) for _ in range(2)]
        Qp = [sb2([64, 3, QB], BF16) for _ in range(2)]
        ksq = sb2([128, QB], BF16)
        ksq2 = sb2([64, QB], BF16)
        kmx = sb2([128, 3, 32])
        kmax = sb2([128, 3])
        qmax = sb2([128, 1])
        nbias = sb2([128, 1])
        PT = [sb2([128, QB], BF16) for _ in range(3)]
        rrs = sb2([128, QB])
        oc = [sb2([128, QB]) for _ in range(2)]
        SP = [PS[1], PS[2]]
        OP = [PS[4], PS[5]]
        RP = [PS[6], PS[7]]
        BQ = PS[3]
        p.op("dve", lambda e: e.memset(kmx[:].ap, 0.0), [], [kmx])
        ngrp = (ntk + QB - 1) // QB
        for hh in range(3):
            for gi in range(ngrp):
                a = gi * QB
                wd_ = min(QB, ntk - a)
                p.I("act", "activation", out=ksq[:, 0:wd_], in_=Kn[:, hh, a:a + wd_], func=AF.Square)
                p.I("act", "activation", out=ksq2[:, 0:wd_], in_=Kp[:, a:a + wd_], func=AF.Square)
                p.mm(BQ[:, 0:wd_], ones_bf[:], ksq[:, 0:wd_], True, False)
                p.mm(BQ[:, 0:wd_], ones_bf[0:64, :], ksq2[:, 0:wd_], False, True)
                p.I("dve", "tensor_reduce", out=kmx[:, hh, gi:gi + 1], in_=BQ[:, 0:wd_], axis=AX.X, op=ALU.max)
            p.I("dve", "tensor_reduce", out=kmax[:, hh:hh + 1], in_=kmx[:, hh, :], axis=AX.X, op=ALU.max)
        qblocks = [(0, CTX, [0, 1])]
        nlat = ntk - CTX
        for a in range(0, nlat, QB):
            qblocks.append((CTX + a, min(QB, nlat - a), list(range(nkt))))
        for qi, (q0, nq, kts) in enumerate(qblocks):
            qn = Qn[qi % 2]; qp = Qp[qi % 2]
            p.dma(qn[:, :, 0:nq], Qsn[:, :, q0:q0 + nq])
            p.dma(qp[:, :, 0:nq], Qsp[:, :, q0:q0 + nq])
            for hh in range(3):
                p.I("act", "activation", out=ksq[:, 0:nq], in_=qn[:, hh, 0:nq], func=AF.Square)
                p.I("act", "activation", out=ksq2[:, 0:nq], in_=qp[:, hh, 0:nq], func=AF.Square)
                p.mm(BQ[:, 0:nq], ones_bf[:], ksq[:, 0:nq], True, False)
                p.mm(BQ[:, 0:nq], ones_bf[0:64, :], ksq2[:, 0:nq], False, True)
                p.I("dve", "tensor_reduce", out=qmax[:], in_=BQ[:, 0:nq], axis=AX.X, op=ALU.max)
                p.I("dve", "tensor_tensor", out=qmax[:], in0=qmax[:], in1=kmax[:, hh:hh + 1], op=ALU.mult)
                p.I("act", "activation", out=nbias[:], in_=qmax[:], func=AF.Sqrt)
                p.I("dve", "tensor_scalar", out=nbias[:], in0=nbias[:], scalar1=-ASCALE, scalar2=None, op0=ALU.mult)
                op_ = OP[hh % 2]; rp_ = RP[hh % 2]
                nk = len(kts)

                def smm(i):
                    kt = kts[i]
                    spb = SP[i % 2]
                    p.mm(spb[:, 0:nq], Kn[:, hh, kt * 128:(kt + 1) * 128], qn[:, hh, 0:nq], True, False)
                    p.mm(spb[:, 0:nq], Kp[:, kt * 128:(kt + 1) * 128], qp[:, hh, 0:nq], False, True)
                smm(0)
                for i in range(nk):
                    if i + 1 < nk:
                        smm(i + 1)
                    pt = PT[i % 3]
                    p.I("act", "activation", out=pt[:, 0:nq], in_=SP[i % 2][:, 0:nq], func=AF.Exp, scale=ASCALE, bias=nbias[:])
                    p.mm(op_[:, 0:nq], Vt[:, kts[i], hh * 128:(hh + 1) * 128], pt[:, 0:nq], i == 0, i == nk - 1)
                    p.mm(rp_[:, 0:nq], ones_bf[:], pt[:, 0:nq], i == 0, i == nk - 1)
                p.I("dve", "reciprocal", out=rrs[:, 0:nq], in_=rp_[:, 0:nq])
                o_ = oc[hh % 2]
                p.I("dve", "tensor_tensor", out=o_[:, 0:nq], in0=op_[:, 0:nq], in1=rrs[:, 0:nq], op=ALU.mult)
                p.dma(outC[hh * 128:(hh + 1) * 128, q0:q0 + nq], o_[:, 0:nq])
        es2.close()

    p.finish()
    return nc, p

import os

D = 2048
EPS = 1e-6
PT = 512


def post_blocks(nctx, nlat):
    blks = []
    if nctx:
        blks.append((0, nctx, 1))
    for a in range(0, nlat, PT):
        blks.append((nctx + a, min(PT, nlat - a), 0))
    return blks


def build_post(kind, nctx, nlat, final, nblk=None, nexp=8, nf_override=None):
    nc = bass.Bass("TRN2", target_bir_lowering=False)
    p = Prog(nc)
    NT = nctx + nlat
    NF = (5632 if kind == "ffn" else 7168) // 128
    if nf_override:
        NF = nf_override
    NE = 1 if kind == "ffn" else nexp
    route = kind == "route"
    if route:
        NE = 0
        NF = 1

    def din(name, shape):
        return nc.dram_tensor(name, list(shape), F32, kind="ExternalInput").ap()

    xT = din("xT", [D, NT])
    aT = din("aT", [512, NT])
    ofT = din("ofT", [768, NT])
    obT = din("obT", [768, NT])
    sgT = din("sgT", [768, NT])
    cT = din("cT", [768, NT])
    c2T = din("c2T", [D, 2])
    ada_w = din("ada_w", [D, 4 * D])
    ada_bT = din("ada_bT", [128, 64])
    n2g = din("n2g", [128, 16])
    dng_d = din("dng", [128, 1])
    fg_d = din("fg", [128, 16])
    w_out = din("w_out", [D, D])
    c_ones = din("c_ones", [128, 128])
    c_ident = din("c_ident", [128, 128])
    if route:
        rt_d = din("router", [D, 8])
        h2T_o = nc.dram_tensor("h2T", [D, NT], F32, kind="ExternalOutput").ap()
        G_o = nc.dram_tensor("G", [NT, 8], F32, kind="ExternalOutput").ap()
    elif kind == "ffn":
        wg_d = [din("wg", [D, NF * 128])]
        wu_d = [din("wu", [D, NF * 128])]
        wd_d = [din("wd", [NF * 128, D])]
    else:
        wg_d = [din(f"wg{e}", [D, NF * 128]) for e in range(NE)]
        wu_d = [din(f"wu{e}", [D, NF * 128]) for e in range(NE)]
        wd_d = [din(f"wd{e}", [NF * 128, D]) for e in range(NE)]
        rt_d = din("router", [D, 8])
    xoT = nc.dram_tensor("xoT", [D, NT], F32, kind="ExternalOutput").ap()

    ones_bf = p.sb([128, 128], BF16)
    p.dma(ones_bf[:], c_ones, q="pool")
    ones_f = p.sb([128, 128])
    p.dma(ones_f[:], c_ones)
    ident_f = p.sb([128, 128])
    p.dma(ident_f[:], c_ident)
    epsb = p.sb([128, 1])
    p.op("dve", lambda e: e.memset(epsb[:].ap, EPS), [], [epsb])
    PS = [p.ps([128, 512]) for _ in range(8)]
    BCp = PS[0]
    Yp = [PS[1], PS[2]]
    Gp = [PS[3], PS[4]]
    Up = [PS[5], PS[6]]
    LG = PS[7]

    x = p.sb([128, 16, PT])
    c2 = p.sb([128, 16, 2])
    p.dma(c2[:], c2T.rearrange("(c p) n -> p c n", p=128))
    sc2 = p.sb([128, 16, 2])
    p.I("act", "activation", out=sc2[:], in_=c2[:], func=AF.Silu)
    adab = p.sb([128, 64])
    p.dma(adab[:], ada_bT)
    n2 = p.sb([128, 16])
    p.dma(n2[:], n2g)
    dng = p.sb([128, 1])
    p.dma(dng[:], dng_d)
    fg = p.sb([128, 16])
    p.dma(fg[:], fg_d)
    modT = p.sb([128, 64, 2])
    awt = [x[:, 0:4, :].rearrange("p a (b c) -> p (a b) c", c=128), x[:, 4:8, :].rearrange("p a (b c) -> p (a b) c", c=128)]
    for j in range(64):
        w = awt[j % 2]
        p.dma(w, ada_w[:, j * 128:(j + 1) * 128].rearrange("(c p) n -> p c n", p=128))
        for k in range(16):
            p.mm(BCp[:, 2 * (j % 2):2 * (j % 2) + 2], w[:, k, :], sc2[:, k, :], k == 0, k == 15)
        p.I("dve", "tensor_scalar", out=modT[:, j, :], in0=BCp[:, 2 * (j % 2):2 * (j % 2) + 2], scalar1=adab[:, j:j + 1],
            scalar2=None, op0=ALU.add)
    gs2 = p.sb([128, 16, 2])
    for s in range(2):
        p.I("dve", "scalar_tensor_tensor", out=gs2[:, :, s], in0=modT[:, 32:48, s], scalar=1.0, in1=n2[:], op0=ALU.add, op1=ALU.mult)
    if kind in ("moe", "route"):
        rt = p.sb([128, 16, 8])
        p.dma(rt[:], rt_d.rearrange("(c p) n -> p c n", p=128))
    p.barrier()

    mix = p.sb([128, 16, PT], BF16)
    h2 = p.sb([128, 16, PT], BF16)
    hid = p.sb([128, NF, PT], BF16)
    ld = [p.sb([128, PT]) for _ in range(4)]
    o32 = p.sb([128, PT])
    sqb = [p.sb([128, PT], BF16) for _ in range(2)]
    rs = p.sb([128, PT])
    tf = [p.sb([128, PT]) for _ in range(2)]
    wt = [p.sb([128, 16, 128], BF16) for _ in range(4)]
    wdt = [p.sb([128, NF, 128], BF16) for _ in range(2)]
    sgt = [p.sb([128, PT]) for _ in range(2)]
    if kind in ("moe", "route"):
        h2f = [p.sb([128, PT]) for _ in range(2)]
        h2ff = p.sb([128, 16, PT]) if route else None
        lg = p.sb([128, 4, 8]); lg2 = p.sb([128, 4, 8])
        eq1 = p.sb([128, 4, 8]); eq2 = p.sb([128, 4, 8])
        m1 = p.sb([128, 4]); m2 = p.sb([128, 4]); dl = p.sb([128, 4]); w1 = p.sb([128, 4]); w2 = p.sb([128, 4])
        gate = p.sb([128, 4, 8])
        gl = [p.sb([128, 128]) for _ in range(2)]
        gbc = p.sb([128, max(NE, 1), PT], BF16)
        sg2 = [p.sb([128, PT]) for _ in range(2)]
    wcnt = [0]
    ldc = [0]

    def nld():
        t = ld[ldc[0] % 4]
        ldc[0] += 1
        return t

    def nwt():
        t = wt[wcnt[0] % 4]
        wcnt[0] += 1
        return t

    blks = post_blocks(nctx, nlat)
    if nblk:
        blks = blks[:nblk]
    for (t0, T, s) in blks:
        for c in range(16):
            p.dma(x[:, c, 0:T], xT[c * 128:(c + 1) * 128, t0:t0 + T])
        for hd in range(6):
            a_ = nld(); b_ = nld(); g_ = nld()
            p.dma(a_[:, 0:T], ofT[hd * 128:(hd + 1) * 128, t0:t0 + T])
            p.dma(b_[:, 0:T], obT[hd * 128:(hd + 1) * 128, t0:t0 + T])
            p.dma(g_[:, 0:T], sgT[hd * 128:(hd + 1) * 128, t0:t0 + T])
            p.I("dve", "tensor_tensor", out=o32[:, 0:T], in0=a_[:, 0:T], in1=b_[:, 0:T], op=ALU.add)
            sq = sqb[hd % 2]
            p.I("act", "activation", out=sq[:, 0:T], in_=o32[:, 0:T], func=AF.Square)
            p.mm(BCp[:, 0:T], ones_bf[:], sq[:, 0:T], True, True)
            p.I("act", "activation", out=rs[:, 0:T], in_=BCp[:, 0:T], func=AF.Sqrt, scale=1.0 / 128, bias=epsb[:])
            p.I("dve", "reciprocal", out=rs[:, 0:T], in_=rs[:, 0:T])
            p.I("dve", "scalar_tensor_tensor", out=o32[:, 0:T], in0=o32[:, 0:T], scalar=dng[:, 0:1], in1=rs[:, 0:T], op0=ALU.mult, op1=ALU.mult)
            p.I("dve", "tensor_tensor", out=mix[:, 4 + hd, 0:T], in0=o32[:, 0:T], in1=g_[:, 0:T], op=ALU.mult)
        for g in range(4):
            a_ = nld()
            p.dma(a_[:, 0:T], aT[g * 128:(g + 1) * 128, t0:t0 + T])
            p.I("act", "activation", out=mix[:, g, 0:T], in_=a_[:, 0:T], func=AF.Identity)
        for hd in range(6):
            a_ = nld()
            p.dma(a_[:, 0:T], cT[hd * 128:(hd + 1) * 128, t0:t0 + T])
            p.I("act", "activation", out=mix[:, 10 + hd, 0:T], in_=a_[:, 0:T], func=AF.Identity)
        for m in range(16):
            w = nwt()
            p.dma(w[:], w_out[:, m * 128:(m + 1) * 128].rearrange("(c p) n -> p c n", p=128), q="pool")
            yp = Yp[m % 2]
            for k in range(16):
                p.mm(yp[:, 0:T], w[:, k, :], mix[:, k, 0:T], k == 0, k == 15)
            p.I("dve", "scalar_tensor_tensor", out=x[:, m, 0:T], in0=yp[:, 0:T], scalar=modT[:, m, s:s + 1], in1=x[:, m, 0:T], op0=ALU.mult, op1=ALU.add)
        for c in range(16):
            sq = sqb[c % 2]
            p.I("act", "activation", out=sq[:, 0:T], in_=x[:, c, 0:T], func=AF.Square)
            p.mm(BCp[:, 0:T], ones_bf[:], sq[:, 0:T], c == 0, c == 15)
        p.I("act", "activation", out=rs[:, 0:T], in_=BCp[:, 0:T], func=AF.Sqrt, scale=1.0 / D, bias=epsb[:])
        p.I("dve", "reciprocal", out=rs[:, 0:T], in_=rs[:, 0:T])
        nsb = T // 128
        for c in range(16):
            t_ = tf[c % 2]
            p.I("dve", "tensor_tensor", out=t_[:, 0:T], in0=x[:, c, 0:T], in1=rs[:, 0:T], op=ALU.mult)
            if kind == "ffn":
                p.I("act", "activation", out=h2[:, c, 0:T], in_=t_[:, 0:T], func=AF.Identity, scale=gs2[:, c, s:s + 1], bias=modT[:, 16 + c, s:s + 1])
            else:
                if route:
                    p.I("act", "activation", out=h2ff[:, c, 0:T], in_=t_[:, 0:T], func=AF.Identity, scale=gs2[:, c, s:s + 1], bias=modT[:, 16 + c, s:s + 1])
                    p.dma(h2T_o[c * 128:(c + 1) * 128, t0:t0 + T], h2ff[:, c, 0:T])
                else:
                    raise NotImplementedError("dense-masked moe path disabled")
        if route:
            for sbk in range(nsb):
                for c in range(16):
                    p.mm(LG[:, sbk * 8:(sbk + 1) * 8], h2ff[:, c, sbk * 128:(sbk + 1) * 128], rt[:, c, :], c == 0, c == 15)
        if kind in ("moe", "route"):
            p.I("dve", "tensor_copy", out=lg[:, 0:nsb, :], in_=LG[:, 0:nsb * 8].rearrange("p (a b) -> p a b", b=8))
            p.I("dve", "tensor_reduce", out=m1[:, 0:nsb], in_=lg[:, 0:nsb, :], axis=AX.X, op=ALU.max)
            for sbk in range(nsb):
                p.I("dve", "tensor_scalar", out=eq1[:, sbk, :], in0=lg[:, sbk, :], scalar1=m1[:, sbk:sbk + 1], scalar2=None, op0=ALU.is_equal)
            p.I("dve", "scalar_tensor_tensor", out=lg2[:, 0:nsb, :], in0=eq1[:, 0:nsb, :], scalar=-1e30, in1=lg[:, 0:nsb, :], op0=ALU.mult, op1=ALU.add)
            p.I("dve", "tensor_reduce", out=m2[:, 0:nsb], in_=lg2[:, 0:nsb, :], axis=AX.X, op=ALU.max)
            for sbk in range(nsb):
                p.I("dve", "tensor_scalar", out=eq2[:, sbk, :], in0=lg2[:, sbk, :], scalar1=m2[:, sbk:sbk + 1], scalar2=None, op0=ALU.is_equal)
            p.I("dve", "tensor_tensor", out=dl[:, 0:nsb], in0=m2[:, 0:nsb], in1=m1[:, 0:nsb], op=ALU.subtract)
            p.I("act", "activation", out=w1[:, 0:nsb], in_=dl[:, 0:nsb], func=AF.Sigmoid, scale=-1.0)
            p.I("act", "activation", out=w2[:, 0:nsb], in_=dl[:, 0:nsb], func=AF.Sigmoid)
            for sbk in range(nsb):
                p.I("dve", "tensor_scalar", out=eq1[:, sbk, :], in0=eq1[:, sbk, :], scalar1=w1[:, sbk:sbk + 1], scalar2=None, op0=ALU.mult)
                p.I("dve", "scalar_tensor_tensor", out=gate[:, sbk, :], in0=eq2[:, sbk, :], scalar=w2[:, sbk:sbk + 1], in1=eq1[:, sbk, :], op0=ALU.mult, op1=ALU.add)
            if route:
                p.dma(G_o[t0:t0 + T, :].rearrange("(a p) e -> p a e", p=128), gate[:, 0:nsb, :])
            for e in range(NE):
                for sbk in range(nsb):
                    g_ = gl[(e * nsb + sbk) % 2]
                    p.I("dve", "tensor_scalar", out=g_[:], in0=ones_f[:], scalar1=gate[:, sbk, e:e + 1], scalar2=None, op0=ALU.mult)
                    p.mm(LG[:, sbk * 128:(sbk + 1) * 128], g_[:], ident_f[:], True, True)
                p.I("act", "activation", out=gbc[:, e, 0:T], in_=LG[:, 0:T], func=AF.Identity)
        for e in range(NE):
            for f in range(NF):
                wg = nwt()
                p.dma(wg[:], wg_d[e][:, f * 128:(f + 1) * 128].rearrange("(c p) n -> p c n", p=128), q="pool")
                wu = nwt()
                p.dma(wu[:], wu_d[e][:, f * 128:(f + 1) * 128].rearrange("(c p) n -> p c n", p=128), q="pool")
                gp = Gp[f % 2]; up = Up[f % 2]
                for k in range(16):
                    p.mm(gp[:, 0:T], wg[:, k, :], h2[:, k, 0:T], k == 0, k == 15)
                for k in range(16):
                    p.mm(up[:, 0:T], wu[:, k, :], h2[:, k, 0:T], k == 0, k == 15)
                st = sgt[f % 2]
                p.I("act", "activation", out=st[:, 0:T], in_=gp[:, 0:T], func=AF.Silu)
                if kind == "moe":
                    s2 = sg2[f % 2]
                    p.I("pool", "tensor_tensor", out=s2[:, 0:T], in0=st[:, 0:T], in1=gbc[:, e, 0:T], op=ALU.mult)
                    p.I("dve", "tensor_tensor", out=hid[:, f, 0:T], in0=s2[:, 0:T], in1=up[:, 0:T], op=ALU.mult)
                else:
                    p.I("dve", "tensor_tensor", out=hid[:, f, 0:T], in0=st[:, 0:T], in1=up[:, 0:T], op=ALU.mult)
            for m in range(16):
                wd = wdt[m % 2]
                nsp = 4
                per = NF // nsp
                for q4 in range(nsp):
                    fa = q4 * per
                    fb = NF if q4 == nsp - 1 else (q4 + 1) * per
                    p.dma(wd[:, fa:fb, :], wd_d[e][fa * 128:fb * 128, m * 128:(m + 1) * 128].rearrange("(c p) n -> p c n", p=128), q="pool")
                yp = Yp[m % 2]
                for f in range(NF):
                    p.mm(yp[:, 0:T], wd[:, f, :], hid[:, f, 0:T], f == 0, f == NF - 1)
                p.I("dve", "scalar_tensor_tensor", out=x[:, m, 0:T], in0=yp[:, 0:T], scalar=modT[:, 48 + m, s:s + 1], in1=x[:, m, 0:T], op0=ALU.mult, op1=ALU.add)
        if final:
            for c in range(16):
                sq = sqb[c % 2]
                p.I("act", "activation", out=sq[:, 0:T], in_=x[:, c, 0:T], func=AF.Square)
                p.mm(BCp[:, 0:T], ones_bf[:], sq[:, 0:T], c == 0, c == 15)
            p.I("act", "activation", out=rs[:, 0:T], in_=BCp[:, 0:T], func=AF.Sqrt, scale=1.0 / D, bias=epsb[:])
            p.I("dve", "reciprocal", out=rs[:, 0:T], in_=rs[:, 0:T])
            for c in range(16):
                p.I("dve", "scalar_tensor_tensor", out=x[:, c, 0:T], in0=x[:, c, 0:T], scalar=fg[:, c:c + 1], in1=rs[:, 0:T], op0=ALU.mult, op1=ALU.mult)
        for c in range(16):
            p.dma(xoT[c * 128:(c + 1) * 128, t0:t0 + T], x[:, c, 0:T])
    p.finish()
    return nc, p


def build_expert(C, NF=56):
    nc = bass.Bass("TRN2", target_bir_lowering=False)
    p = Prog(nc)

    def din(name, shape):
        return nc.dram_tensor(name, list(shape), F32, kind="ExternalInput").ap()
    hT = din("hT", [D, C])
    gwb_d = din("gwb", [128, C])
    wg_d = din("wg", [D, NF * 128])
    wu_d = din("wu", [D, NF * 128])
    wd_d = din("wd", [NF * 128, D])
    yT = nc.dram_tensor("yT", [D, C], F32, kind="ExternalOutput").ap()
    PS = [p.ps([128, 512]) for _ in range(8)]
    Yp = [PS[0], PS[1]]
    Gp = [PS[2], PS[3]]
    Up = [PS[4], PS[5]]
    P2 = 2 * PT
    h2 = p.sb([128, 16, P2], BF16)
    hid = p.sb([128, NF, P2], BF16)
    ld = [p.sb([128, PT]) for _ in range(3)]
    gwb = p.sb([128, P2])
    wt = [p.sb([128, 16, 128], BF16) for _ in range(3)]
    wdt = [p.sb([128, NF, 128], BF16) for _ in range(1)]
    sgt = [p.sb([128, PT]) for _ in range(2)]
    sg2 = [p.sb([128, PT]) for _ in range(2)]
    yo = [p.sb([128, PT]) for _ in range(2)]
    wcnt = 0
    lc = 0
    for t0 in range(0, C, P2):
        T2 = min(P2, C - t0)
        halves = [(a, min(PT, T2 - a)) for a in range(0, T2, PT)]
        for c in range(16):
            for (a, T) in halves:
                a_ = ld[lc % 3]; lc += 1
                p.dma(a_[:, 0:T], hT[c * 128:(c + 1) * 128, t0 + a:t0 + a + T])
                p.I("act", "activation", out=h2[:, c, a:a + T], in_=a_[:, 0:T], func=AF.Identity)
        p.dma(gwb[:, 0:T2], gwb_d[:, t0:t0 + T2])
        for f in range(NF):
            wg = wt[wcnt % 3]; wcnt += 1
            p.dma(wg[:], wg_d[:, f * 128:(f + 1) * 128].rearrange("(c p) n -> p c n", p=128), q="pool")
            wu = wt[wcnt % 3]; wcnt += 1
            p.dma(wu[:], wu_d[:, f * 128:(f + 1) * 128].rearrange("(c p) n -> p c n", p=128), q="pool")
            for hi, (a, T) in enumerate(halves):
                gp = Gp[hi]; up = Up[hi]
                for k in range(16):
                    p.mm(gp[:, 0:T], wg[:, k, :], h2[:, k, a:a + T], k == 0, k == 15)
                for k in range(16):
                    p.mm(up[:, 0:T], wu[:, k, :], h2[:, k, a:a + T], k == 0, k == 15)
                st = sgt[hi]
                p.I("act", "activation", out=st[:, 0:T], in_=gp[:, 0:T], func=AF.Silu)
                s2 = sg2[hi]
                p.I("pool", "tensor_tensor", out=s2[:, 0:T], in0=st[:, 0:T], in1=gwb[:, a:a + T], op=ALU.mult)
                p.I("dve", "tensor_tensor", out=hid[:, f, a:a + T], in0=s2[:, 0:T], in1=up[:, 0:T], op=ALU.mult)
        for m in range(16):
            wd = wdt[0]
            nsp = 4
            per = NF // nsp
            for q4 in range(nsp):
                fa = q4 * per
                fb = NF if q4 == nsp - 1 else (q4 + 1) * per
                p.dma(wd[:, fa:fb, :], wd_d[fa * 128:fb * 128, m * 128:(m + 1) * 128].rearrange("(c p) n -> p c n", p=128), q="pool")
            for hi, (a, T) in enumerate(halves):
                yp = Yp[hi]
                for f in range(NF):
                    p.mm(yp[:, 0:T], wd[:, f, :], hid[:, f, a:a + T], f == 0, f == NF - 1)
                y_ = yo[hi]
                p.I("act", "activation", out=y_[:, 0:T], in_=yp[:, 0:T], func=AF.Identity)
                p.dma(yT[m * 128:(m + 1) * 128, t0 + a:t0 + a + T], y_[:, 0:T])
    p.finish()
    return nc, p


def build_combine(NT):
    nc = bass.Bass("TRN2", target_bir_lowering=False)
    p = Prog(nc)

    def din(name, shape):
        return nc.dram_tensor(name, list(shape), F32, kind="ExternalInput").ap()
    xT = din("xT", [D, NT])
    y1T = din("y1T", [D, NT])
    y2T = din("y2T", [D, NT])
    c2T = din("c2T", [D, 2])
    ada_w = din("ada_w", [D, D])
    ada_bT = din("ada_bT", [128, 16])
    fg_d = din("fg", [128, 16])
    c_ones = din("c_ones", [128, 128])
    oT = nc.dram_tensor("oT", [D, NT], F32, kind="ExternalOutput").ap()
    ones_bf = p.sb([128, 128], BF16)
    p.dma(ones_bf[:], c_ones, q="pool")
    epsb = p.sb([128, 1])
    p.op("dve", lambda e: e.memset(epsb[:].ap, EPS), [], [epsb])
    PS = [p.ps([128, 512]) for _ in range(2)]
    BCp = PS[0]
    x = p.sb([128, 16, PT])
    c2 = p.sb([128, 16, 2])
    p.dma(c2[:], c2T.rearrange("(c p) n -> p c n", p=128))
    sc2 = p.sb([128, 16, 2])
    p.I("act", "activation", out=sc2[:], in_=c2[:], func=AF.Silu)
    adab = p.sb([128, 16])
    p.dma(adab[:], ada_bT)
    fg = p.sb([128, 16])
    p.dma(fg[:], fg_d)
    modT = p.sb([128, 16, 2])
    awt = [x[:, 0:4, :].rearrange("p a (b c) -> p (a b) c", c=128), x[:, 4:8, :].rearrange("p a (b c) -> p (a b) c", c=128)]
    for j in range(16):
        w = awt[j % 2]
        p.dma(w, ada_w[:, j * 128:(j + 1) * 128].rearrange("(c p) n -> p c n", p=128))
        for k in range(16):
            p.mm(PS[1][:, 2 * (j % 2):2 * (j % 2) + 2], w[:, k, :], sc2[:, k, :], k == 0, k == 15)
        p.I("dve", "tensor_scalar", out=modT[:, j, :], in0=PS[1][:, 2 * (j % 2):2 * (j % 2) + 2], scalar1=adab[:, j:j + 1],
            scalar2=None, op0=ALU.add)
    p.barrier()
    ld = [p.sb([128, PT]) for _ in range(4)]
    ys = [p.sb([128, PT]) for _ in range(2)]
    sqb = [p.sb([128, PT], BF16) for _ in range(2)]
    rs = p.sb([128, PT])
    for t0 in range(0, NT, PT):
        T = min(PT, NT - t0)
        for c in range(16):
            a_ = ld[(2 * c) % 4]; b_ = ld[(2 * c + 1) % 4]
            p.dma(x[:, c, 0:T], xT[c * 128:(c + 1) * 128, t0:t0 + T])
            p.dma(a_[:, 0:T], y1T[c * 128:(c + 1) * 128, t0:t0 + T])
            p.dma(b_[:, 0:T], y2T[c * 128:(c + 1) * 128, t0:t0 + T])
            y_ = ys[c % 2]
            p.I("dve", "tensor_tensor", out=y_[:, 0:T], in0=a_[:, 0:T], in1=b_[:, 0:T], op=ALU.add)
            p.I("dve", "scalar_tensor_tensor", out=x[:, c, 0:T], in0=y_[:, 0:T], scalar=modT[:, c, 0:1], in1=x[:, c, 0:T], op0=ALU.mult, op1=ALU.add)
            sq = sqb[c % 2]
            p.I("act", "activation", out=sq[:, 0:T], in_=x[:, c, 0:T], func=AF.Square)
            p.mm(BCp[:, 0:T], ones_bf[:], sq[:, 0:T], c == 0, c == 15)
        p.I("act", "activation", out=rs[:, 0:T], in_=BCp[:, 0:T], func=AF.Sqrt, scale=1.0 / D, bias=epsb[:])
        p.I("dve", "reciprocal", out=rs[:, 0:T], in_=rs[:, 0:T])
        for c in range(16):
            p.I("dve", "scalar_tensor_tensor", out=x[:, c, 0:T], in0=x[:, c, 0:T], scalar=fg[:, c:c + 1], in1=rs[:, 0:T], op0=ALU.mult, op1=ALU.mult)
            p.dma(oT[c * 128:(c + 1) * 128, t0:t0 + T], x[:, c, 0:T])
    p.finish()
    return nc, p

def _unflip(M):
    return np.concatenate([M[:, :CTX][:, ::-1], M[:, CTX:][:, ::-1]], axis=1)


def mix_host2(inp, li, r, XT_b, CT_b):
    b, dr = r // 2, r % 2
    f32 = np.float32
    xs = XT_b[:, ::-1] if dr else XT_b
    cs = CT_b[:, ::-1] if dr else CT_b
    xT = np.zeros((D, NCOL), f32)
    xT[:, 2:2 + CTX] = cs
    xT[:, NB + 2:NB + 2 + SEQ] = xs
    dummy = np.zeros((1, SEQ, 1), f32)
    m = mix_host(inp, li, r, None, None, xT_ready=xT)
    return m


def _run(nc, in_maps):
    res = run_bass_kernel_spmd(nc, in_maps, core_ids=list(range(len(in_maps))))
    return res.results


def kernel(**inputs):
    inp = {k: np.asarray(v) for k, v in inputs.items()}
    f32 = np.float32
    NB_ = 4
    XT = [np.ascontiguousarray(inp["x"][b].T) for b in range(NB_)]
    CTs = [np.ascontiguousarray(inp["ctx"][b].T) for b in range(NB_)]
    consts = make_consts()
    nc_mix, _ = build_mix(("sgu", "dn", "mla", "attn"), NBLK)
    HL = SEQ // 2
    HC = CTX // 2
    out_full = np.zeros((NB_, SEQ, D), f32)
    for li in range(2):
        in_maps = [mix_host2(inp, li, r, XT[r // 2], CTs[r // 2]) for r in range(8)]
        res = _run(nc_mix, in_maps)
        del in_maps
        post_in = []
        for r in range(8):
            b, hf = r // 2, r % 2
            A, B = res[2 * b], res[2 * b + 1]
            if li == 0:
                cols = np.concatenate([np.arange(hf * HC, (hf + 1) * HC), CTX + np.arange(hf * HL, (hf + 1) * HL)])
                xin = np.concatenate([CTs[b][:, hf * HC:(hf + 1) * HC], XT[b][:, hf * HL:(hf + 1) * HL]], axis=1)
            else:
                cols = CTX + np.arange(hf * HL, (hf + 1) * HL)
                xin = XT[b][:, hf * HL:(hf + 1) * HL]
            m = {}
            m["xT"] = np.ascontiguousarray(xin)
            m["aT"] = np.ascontiguousarray(np.concatenate([A["outA"][:, cols], _unflip(B["outA"])[:, cols]], axis=0))
            m["ofT"] = np.ascontiguousarray(A["oT"][:, cols])
            m["obT"] = np.ascontiguousarray(_unflip(B["oT"])[:, cols])
            m["sgT"] = np.ascontiguousarray(np.concatenate([A["sgate"][:, cols], _unflip(B["sgate"])[:, cols]], axis=0))
            m["cT"] = np.ascontiguousarray(np.concatenate([A["outC"][:, cols], _unflip(B["outC"])[:, cols]], axis=0))
            m["c2T"] = np.ascontiguousarray(np.stack([inp["c"][b], inp["c_ctx"]], axis=1))
            m["ada_w"] = np.ascontiguousarray(inp["ada_w"][li][:, 2 * D:])
            m["ada_bT"] = np.ascontiguousarray(inp["ada_b"][li][2 * D:].reshape(64, 128).T)
            m["n2g"] = np.ascontiguousarray(inp["norm2_g"][li].reshape(16, 128).T)
            m["dng"] = np.ascontiguousarray(inp["dn_norm_g"][li].reshape(128, 1))
            m["fg"] = np.ascontiguousarray(inp["final_norm_g"].reshape(16, 128).T)
            m["w_out"] = np.ascontiguousarray(inp["w_out"][li])
            m["c_ones"] = consts["ones"]
            m["c_ident"] = consts["ident"]
            if li == 0:
                m["wg"] = np.ascontiguousarray(inp["ffn_w_gate"][0])
                m["wu"] = np.ascontiguousarray(inp["ffn_w_up"][0])
                m["wd"] = np.ascontiguousarray(inp["ffn_w_down"][0])
            else:
                m["router"] = np.ascontiguousarray(inp["moe_router"][0])
            post_in.append(m)
        del res
        if li == 0:
            nc_p, _ = build_post("ffn", HC, HL, False)
            pres = _run(nc_p, post_in)
            del post_in
            for r in range(8):
                b, hf = r // 2, r % 2
                xo = pres[r]["xoT"]
                CTs[b][:, hf * HC:(hf + 1) * HC] = xo[:, :HC]
                XT[b][:, hf * HL:(hf + 1) * HL] = xo[:, HC:]
            del pres
        else:
            nc_p, _ = build_post("route", 0, HL, False)
            pres = _run(nc_p, post_in)
            del post_in
            NTOK_ALL = 8 * HL
            Gall = np.concatenate([pres[r]["G"] for r in range(8)], axis=0)
            H2 = np.concatenate([pres[r]["h2T"] for r in range(8)], axis=1)
            xmid = [pres[r]["xoT"] for r in range(8)]
            del pres
            idx = [np.nonzero(Gall[:, e])[0] for e in range(8)]
            cmax = max(len(i) for i in idx)
            C = ((cmax + 511) // 512) * 512
            ex_in = []
            for e in range(8):
                n = len(idx[e])
                hT = np.zeros((D, C), f32)
                hT[:, :n] = H2[:, idx[e]]
                gwb = np.zeros((128, C), f32)
                gwb[:, :n] = np.broadcast_to(Gall[idx[e], e][None, :], (128, n))
                ex_in.append({"hT": hT, "gwb": gwb,
                              "wg": np.ascontiguousarray(inp["moe_w_gate"][0][e]),
                              "wu": np.ascontiguousarray(inp["moe_w_up"][0][e]),
                              "wd": np.ascontiguousarray(inp["moe_w_down"][0][e])})
            del H2
            nc_e, _ = build_expert(C)
            eres = _run(nc_e, ex_in)
            del ex_in
            Y1 = np.zeros((D, NTOK_ALL), f32)
            Y2 = np.zeros((D, NTOK_ALL), f32)
            seen = np.zeros(NTOK_ALL, bool)
            for e in range(8):
                n = len(idx[e])
                cols = idx[e]
                y = eres[e]["yT"][:, :n]
                first = ~seen[cols]
                Y1[:, cols[first]] = y[:, first]
                Y2[:, cols[~first]] = y[:, ~first]
                seen[cols] = True
            del eres
            cb_in = []
            for r in range(8):
                b = r // 2
                cb_in.append({"xT": xmid[r], "y1T": np.ascontiguousarray(Y1[:, r * HL:(r + 1) * HL]),
                              "y2T": np.ascontiguousarray(Y2[:, r * HL:(r + 1) * HL]),
                              "c2T": np.ascontiguousarray(np.stack([inp["c"][b], inp["c_ctx"]], axis=1)),
                              "ada_w": np.ascontiguousarray(inp["ada_w"][1][:, 5 * D:]),
                              "ada_bT": np.ascontiguousarray(inp["ada_b"][1][5 * D:].reshape(16, 128).T),
                              "fg": np.ascontiguousarray(inp["final_norm_g"].reshape(16, 128).T),
                              "c_ones": consts["ones"]})
            nc_c, _ = build_combine(HL)
            cres = _run(nc_c, cb_in)
            for r in range(8):
                b, hf = r // 2, r % 2
                out_full[b, hf * HL:(hf + 1) * HL, :] = cres[r]["oT"].T
    return out_full
```
